# Optimizing a Trainium2 kernel written in Bass

```python
import math
import jax, jax.numpy as jnp
from jax import lax
import numpy as np

D_MODEL = 2048
BATCH = 4
SEQ = 2048
DEPTH = 1

MIX_WIDTH = D_MODEL
DA_WIDTH = MIX_WIDTH // 2
RET_WIDTH = MIX_WIDTH - DA_WIDTH
DA_HEAD_DIM = 64
DA_V_DIM = 2 * DA_HEAD_DIM
DA_HEADS = DA_WIDTH // DA_V_DIM
RET_HEADS = 4
RET_V_DIM = RET_WIDTH // RET_HEADS
RET_K_DIM = RET_V_DIM // 2
RET_CHUNK = 128
Q_BLOCK = 128
ROPE_THETA = 10000.0
PLE_DIM = 256
N_GROUPS = 4
EXPERTS_PER_GROUP = 8
N_EXPERTS = N_GROUPS * EXPERTS_PER_GROUP
TOP_K = 2
EXPERT_FF = D_MODEL // 4
DEEPNORM_ALPHA = (2 * DEPTH) ** 0.25
DEEPNORM_BETA = (8 * DEPTH) ** -0.25
LN_EPS = 1e-5
SPLITS = (DA_HEADS * 2 * DA_HEAD_DIM, DA_HEADS * 2 * DA_HEAD_DIM, DA_HEADS * DA_V_DIM,
          RET_HEADS * RET_K_DIM, RET_HEADS * RET_K_DIM, RET_HEADS * RET_V_DIM, RET_WIDTH)
IN_WIDTH = sum(SPLITS)

kernel_name = "hymba_diffattn_retnet_hiermoe_deepnorm_ple"


def _layer_norm(x, g, b):
    xf = x.astype(jnp.float32)
    mu = jnp.mean(xf, -1, keepdims=True)
    var = jnp.mean(jnp.square(xf - mu), -1, keepdims=True)
    y = (xf - mu) * lax.rsqrt(var + LN_EPS) * g.astype(jnp.float32) + b.astype(jnp.float32)
    return y.astype(x.dtype)


def _rope(t, positions):
    d = t.shape[-1]
    inv = jnp.power(ROPE_THETA, -jnp.arange(0, d, 2, dtype=jnp.float32) / d)
    ang = positions.astype(jnp.float32)[..., None] * inv
    ang = ang.reshape(ang.shape[:2] + (1,) * (t.ndim - 3) + ang.shape[-1:])
    cos, sin = jnp.cos(ang), jnp.sin(ang)
    t1, t2 = jnp.split(t.astype(jnp.float32), 2, axis=-1)
    return jnp.concatenate([t1 * cos - t2 * sin, t2 * cos + t1 * sin], -1).astype(t.dtype)


def _diff_attention(q, k, v, lam, subln_w, lam_init):
    B, S, H, _, dh = q.shape
    dv = v.shape[-1]
    nb = S // Q_BLOCK
    scale = dh ** -0.5
    qb = q.reshape(B, nb, Q_BLOCK, H, 2, dh).transpose(1, 0, 3, 4, 2, 5)
    kt = k.transpose(0, 2, 3, 1, 4)
    vt = v.transpose(0, 2, 1, 3)
    kpos = jnp.arange(S)
    neg = jnp.finfo(jnp.float32).min

    def block(args):
        qblk, i = args
        s = jnp.einsum('bhtqd,bhtkd->bhtqk', qblk, kt).astype(jnp.float32) * scale
        qpos = i * Q_BLOCK + jnp.arange(Q_BLOCK)
        s = jnp.where(kpos[None, :] <= qpos[:, None], s, neg)
        a = jax.nn.softmax(s, axis=-1)
        a = a[:, :, 0] - lam * a[:, :, 1]
        return jnp.einsum('bhqk,bhkd->bhqd', a.astype(v.dtype), vt)

    o = lax.map(block, (qb, jnp.arange(nb)))
    o = o.transpose(1, 0, 3, 2, 4).reshape(B, S, H, dv).astype(jnp.float32)
    o = o * lax.rsqrt(jnp.mean(jnp.square(o), -1, keepdims=True) + LN_EPS)
    o = o * subln_w.astype(jnp.float32) * (1.0 - lam_init)
    return o.reshape(B, S, H * dv).astype(v.dtype)


def _retention(q, k, v, g):
    B, S, H, dk = q.shape
    dv = v.shape[-1]
    C = RET_CHUNK
    nc = S // C
    k = k * (dk ** -0.5)
    to_chunks = lambda t: t.reshape(B, nc, C, H, t.shape[-1]).transpose(0, 3, 1, 2, 4)
    qc, kc, vc = to_chunks(q), to_chunks(k), to_chunks(v)
    lg = jnp.log(1.0 - jnp.power(2.0, -5.0 - jnp.arange(H, dtype=jnp.float32)))
    n = jnp.arange(C, dtype=jnp.float32)
    rel = n[:, None] - n[None, :]
    decay = jnp.where(rel >= 0, jnp.exp(rel[None] * lg[:, None, None]), 0.0)
    inner = jnp.einsum('bhcnd,bhcmd->bhcnm', qc, kc).astype(jnp.float32) * decay[None, :, None]
    inner_o = jnp.einsum('bhcnm,bhcme->bhcne', inner, vc.astype(jnp.float32))
    zeta = jnp.exp((C - 1.0 - n)[None] * lg[:, None])
    xi = jnp.exp((n + 1.0)[None] * lg[:, None])
    kv = jnp.einsum('bhcmd,bhcme->bhcde',
                    kc.astype(jnp.float32) * zeta[None, :, None, :, None],
                    vc.astype(jnp.float32))
    chunk_decay = jnp.exp(C * lg)[None, :, None, None]

    def step(state, kv_c):
        return chunk_decay * state + kv_c, state

    _, r_prev = lax.scan(step, jnp.zeros((B, H, dk, dv), jnp.float32),
                         kv.transpose(2, 0, 1, 3, 4))
    cross = jnp.einsum('bhcnd,cbhde->bhcne', qc.astype(jnp.float32), r_prev)
    y = inner_o + cross * xi[None, :, None, :, None]
    y = y.transpose(0, 2, 3, 1, 4).reshape(B, S, H, dv)
    mu = jnp.mean(y, -1, keepdims=True)
    var = jnp.mean(jnp.square(y - mu), -1, keepdims=True)
    y = ((y - mu) * lax.rsqrt(var + LN_EPS)).reshape(B, S, H * dv)
    return (jax.nn.silu(g.astype(jnp.float32)) * y).astype(v.dtype)


def _hybrid_mixer(x, positions, w_in, w_out, lq1, lk1, lq2, lk2, subln_w, lam_init):
    B, S, _ = x.shape
    q_da, k_da, v_da, q_r, k_r, v_r, g_r = jnp.split(x @ w_in, np.cumsum(SPLITS)[:-1], axis=-1)
    q_da = _rope(q_da.reshape(B, S, DA_HEADS, 2, DA_HEAD_DIM), positions)
    k_da = _rope(k_da.reshape(B, S, DA_HEADS, 2, DA_HEAD_DIM), positions)
    v_da = v_da.reshape(B, S, DA_HEADS, DA_V_DIM)
    f32 = jnp.float32
    lam = (jnp.exp(jnp.sum(lq1.astype(f32) * lk1.astype(f32)))
           - jnp.exp(jnp.sum(lq2.astype(f32) * lk2.astype(f32))) + lam_init)
    o_da = _diff_attention(q_da, k_da, v_da, lam, subln_w, lam_init)
    q_r = _rope(q_r.reshape(B, S, RET_HEADS, RET_K_DIM), positions)
    k_r = _rope(k_r.reshape(B, S, RET_HEADS, RET_K_DIM), positions)
    v_r = v_r.reshape(B, S, RET_HEADS, RET_V_DIM)
    o_r = _retention(q_r, k_r, v_r, g_r)
    return jnp.concatenate([o_da, o_r], axis=-1) @ w_out


def _hier_moe(x, w_rg, b_rg, w_re, b_re, w_gate, w_up, w_down):
    B, S, D = x.shape
    xf = x.reshape(B * S, D)
    gl = (xf @ w_rg).astype(jnp.float32) + b_rg.astype(jnp.float32)
    gp = jax.nn.softmax(gl, axis=-1)
    g_idx = jnp.argmax(gl, axis=-1)
    g_w = jnp.take_along_axis(gp, g_idx[:, None], axis=1)
    el = ((xf @ w_re).astype(jnp.float32) + b_re.astype(jnp.float32)).reshape(-1, N_GROUPS, EXPERTS_PER_GROUP)
    el = jnp.take_along_axis(el, g_idx[:, None, None], axis=1)[:, 0]
    tv, ti = lax.top_k(el, TOP_K)
    fw = jax.nn.softmax(tv, axis=-1) * g_w
    eid = g_idx[:, None] * EXPERTS_PER_GROUP + ti
    combine = jnp.sum(jax.nn.one_hot(eid, N_EXPERTS, dtype=jnp.float32) * fw[..., None], axis=1)

    def expert_step(y, params):
        wg, wu, wd, c = params
        h = jax.nn.silu(xf @ wg) * (xf @ wu)
        return y + (h @ wd).astype(jnp.float32) * c[:, None], None

    y, _ = lax.scan(expert_step, jnp.zeros(xf.shape, jnp.float32),
                    (w_gate, w_up, w_down, combine.T))
    return y.reshape(B, S, D).astype(x.dtype)


def setup_inputs(seed: int = 0) -> dict:
    key = jax.random.key(seed)
    ks = jax.random.split(key, 24)
    nrm = lambda k, shape, s: jax.random.normal(k, shape, jnp.float32) * s
    beta = DEEPNORM_BETA
    col_scale = jnp.concatenate([
        jnp.ones((SPLITS[0] + SPLITS[1],), jnp.float32),
        jnp.full((SPLITS[2],), beta, jnp.float32),
        jnp.ones((SPLITS[3] + SPLITS[4],), jnp.float32),
        jnp.full((SPLITS[5],), beta, jnp.float32),
        jnp.ones((SPLITS[6],), jnp.float32)])
    return {
        "x": nrm(ks[0], (BATCH, SEQ, D_MODEL), 1.0),
        "p": nrm(ks[1], (DEPTH, BATCH, SEQ, PLE_DIM), 1.0),
        "positions": jnp.broadcast_to(jnp.arange(SEQ, dtype=jnp.int32), (BATCH, SEQ)),
        "w_in": nrm(ks[2], (DEPTH, D_MODEL, IN_WIDTH), D_MODEL ** -0.5) * col_scale,
        "w_out": nrm(ks[3], (DEPTH, MIX_WIDTH, D_MODEL), beta * MIX_WIDTH ** -0.5),
        "da_lambda_q1": nrm(ks[4], (DEPTH, DA_HEAD_DIM), 0.1),
        "da_lambda_k1": nrm(ks[5], (DEPTH, DA_HEAD_DIM), 0.1),
        "da_lambda_q2": nrm(ks[6], (DEPTH, DA_HEAD_DIM), 0.1),
        "da_lambda_k2": nrm(ks[7], (DEPTH, DA_HEAD_DIM), 0.1),
        "da_subln_w": 1.0 + nrm(ks[8], (DEPTH, DA_V_DIM), 0.02),
        "ln1_g": 1.0 + nrm(ks[9], (DEPTH, D_MODEL), 0.02),
        "ln1_b": nrm(ks[10], (DEPTH, D_MODEL), 0.02),
        "w_router_group": nrm(ks[11], (DEPTH, D_MODEL, N_GROUPS), D_MODEL ** -0.5),
        "b_router_group": nrm(ks[12], (DEPTH, N_GROUPS), 0.01),
        "w_router_expert": nrm(ks[13], (DEPTH, D_MODEL, N_EXPERTS), D_MODEL ** -0.5),
        "b_router_expert": nrm(ks[14], (DEPTH, N_EXPERTS), 0.01),
        "w_exp_gate": nrm(ks[15], (DEPTH, N_EXPERTS, D_MODEL, EXPERT_FF), D_MODEL ** -0.5),
        "w_exp_up": nrm(ks[16], (DEPTH, N_EXPERTS, D_MODEL, EXPERT_FF), D_MODEL ** -0.5),
        "w_exp_down": nrm(ks[17], (DEPTH, N_EXPERTS, EXPERT_FF, D_MODEL), beta * EXPERT_FF ** -0.5),
        "ln2_g": 1.0 + nrm(ks[18], (DEPTH, D_MODEL), 0.02),
        "ln2_b": nrm(ks[19], (DEPTH, D_MODEL), 0.02),
        "w_ple_gate": nrm(ks[20], (DEPTH, D_MODEL, D_MODEL), D_MODEL ** -0.5),
        "w_ple_proj": nrm(ks[21], (DEPTH, PLE_DIM, D_MODEL), PLE_DIM ** -0.5),
    }


def reference(x, p, positions, w_in, w_out, da_lambda_q1, da_lambda_k1, da_lambda_q2,
              da_lambda_k2, da_subln_w, ln1_g, ln1_b, w_router_group, b_router_group,
              w_router_expert, b_router_expert, w_exp_gate, w_exp_up, w_exp_down,
              ln2_g, ln2_b, w_ple_gate, w_ple_proj):
    for l in range(DEPTH):
        lam_init = 0.8 - 0.6 * math.exp(-0.3 * l)
        h = _hybrid_mixer(x, positions, w_in[l], w_out[l], da_lambda_q1[l], da_lambda_k1[l],
                          da_lambda_q2[l], da_lambda_k2[l], da_subln_w[l], lam_init)
        x = _layer_norm(DEEPNORM_ALPHA * x + h, ln1_g[l], ln1_b[l])
        m = _hier_moe(x, w_router_group[l], b_router_group[l], w_router_expert[l],
                      b_router_expert[l], w_exp_gate[l], w_exp_up[l], w_exp_down[l])
        x = _layer_norm(DEEPNORM_ALPHA * x + m, ln2_g[l], ln2_b[l])
        x = x + jax.nn.sigmoid(x @ w_ple_gate[l]) * (p[l] @ w_ple_proj[l])
    return x
```

```python
import math
from contextlib import ExitStack

import numpy as np
import concourse.bass as bass
import concourse.mybir as mybir
from concourse.bass_utils import run_bass_kernel_spmd

F32 = mybir.dt.float32
BF16 = mybir.dt.bfloat16
I32 = mybir.dt.int32
AF = mybir.ActivationFunctionType
ALU = mybir.AluOpType
AX = mybir.AxisListType

D = 2048
S = 2048
NB = 4
NKC = 16
OWN = 1024
ALPHA = 2.0 ** 0.25
LAM_INIT = 0.8 - 0.6
EPS = 1e-5
THETA = 10000.0
NE = 32
ENGS = ("sync", "act", "dve", "pool", "pe")
STRICT_SAME_ENGINE = True
SB_BASE = 16512
SB_END = 16512 + 212800


class DS:
    __slots__ = ("sem",)

    def __init__(self):
        self.sem = None


class Tk:
    __slots__ = ("name", "w", "r", "ds")

    def __init__(self, name, ds=None):
        self.name = name
        self.w = None
        self.r = []
        self.ds = ds if ds is not None else DS()


class Op:
    __slots__ = ("eng", "fn", "deps", "is_dma", "tk", "signal", "ticket", "idx")


class Prog:
    def __init__(self, nc, esem=None):
        self.nc = nc
        self.ops = []
        self.esem = esem
        self.per_eng = None

    def op(self, eng, fn, reads=(), writes=(), dma=False, signal=False):
        o = Op()
        o.eng = eng
        o.fn = fn
        o.is_dma = dma
        o.idx = len(self.ops)
        o.signal = dma or signal
        o.ticket = None
        o.tk = None
        deps = {}
        for t in reads:
            if t.w is not None:
                deps[t.w] = True
        for t in writes:
            if t.w is not None:
                deps.setdefault(t.w, False)
            for r in t.r:
                deps.setdefault(r, False)
        deps.pop(o.idx, None)
        o.deps = deps
        for t in reads:
            t.r.append(o.idx)
        for t in writes:
            t.w = o.idx
            t.r = []
        if dma:
            o.tk = writes[0]
        self.ops.append(o)
        return o

    def prepare(self):
        nc = self.nc
        ops = self.ops
        for o in ops:
            for d, is_raw in o.deps.items():
                s = ops[d]
                if s.is_dma:
                    continue
                if s.eng == o.eng and not is_raw and (o.eng == "pe" or not STRICT_SAME_ENGINE):
                    continue
                s.signal = True
        if self.esem is None:
            self.esem = {e: nc.alloc_semaphore(name=f"s_{e}_{id(self) % 9973}") for e in ENGS}
        ecount = {e: 0 for e in ENGS}
        dcount = {}
        for o in ops:
            if o.is_dma:
                ds = o.tk.ds
                if ds.sem is None:
                    ds.sem = nc.alloc_semaphore(name=f"d_{id(ds) % 99991}")
                dcount[id(ds)] = dcount.get(id(ds), 0) + 16
                o.ticket = (ds.sem, dcount[id(ds)])
            elif o.signal:
                ecount[o.eng] += 1
                o.ticket = (self.esem[o.eng], ecount[o.eng])
        self.per_eng = {e: [o for o in ops if o.eng == e] for e in ENGS}

    def run_engine(self, e, h):
        ops = self.ops
        clock = {}
        for o in self.per_eng[e]:
            need = {}
            for d, is_raw in o.deps.items():
                s = ops[d]
                if s.eng == e and not s.is_dma and not is_raw and (e == "pe" or not STRICT_SAME_ENGINE):
                    continue
                sem, val = s.ticket
                k = id(sem)
                if clock.get(k, 0) >= val:
                    continue
                if k not in need or need[k][1] < val:
                    need[k] = (sem, val)
            for k, (sem, val) in need.items():
                h.wait_ge(sem, val)
                clock[k] = val
            ins = o.fn(h)
            if o.ticket is not None:
                ins.then_inc(o.ticket[0], 16 if o.is_dma else 1)

    def emit(self, final_wait_ops=()):
        nc = self.nc
        self.prepare()
        finals = [self.ops[i].ticket for i in final_wait_ops]
        P = self

        with nc.Block() as block:
            @block.sync
            def _(h):
                P.run_engine("sync", h)
                for sem, val in finals:
                    h.wait_ge(sem, val)

            @block.scalar
            def _(h):
                P.run_engine("act", h)

            @block.vector
            def _(h):
                P.run_engine("dve", h)

            @block.gpsimd
            def _(h):
                P.run_engine("pool", h)

            @block.tensor
            def _(h):
                P.run_engine("pe", h)
        return {e: len(self.per_eng[e]) for e in ENGS}


class Arena:
    def __init__(self, nc):
        self.nc = nc
        self.big = nc.alloc_sbuf_tensor("big", [128, (SB_END - SB_BASE) // 4], F32)
        self.ptr = SB_BASE

    def at(self, off, shape, dt):
        esz = 2 if dt == BF16 else 4
        n = 1
        for s in shape[1:]:
            n *= s
        nbytes = n * esz
        assert off % 4 == 0 and nbytes % 4 == 0
        assert off >= SB_BASE and off + nbytes <= SB_END, f"SBUF overflow {off}+{nbytes}"
        a = (off - SB_BASE) // 4
        v = self.big[:, a:a + nbytes // 4]
        if dt != F32:
            v = v.bitcast(dt)
        if len(shape) == 3:
            v = v.rearrange("p (a b) -> p a b", a=shape[1])
        elif len(shape) == 4:
            v = v.rearrange("p (a b c) -> p a b c", a=shape[1], b=shape[2])
        return v

    def alloc(self, shape, dt):
        esz = 2 if dt == BF16 else 4
        n = esz
        for s in shape[1:]:
            n *= s
        self.ptr = (self.ptr + 63) // 64 * 64
        off = self.ptr
        self.ptr += (n + 3) // 4 * 4
        return self.at(off, shape, dt)


def build(dbg=None, force_dense=False):
    nc = bass.Bass("TRN2", target_bir_lowering=False)
    dram = lambda n, shp, dt=F32, kind="ExternalInput": nc.dram_tensor(n, list(shp), dt, kind=kind).ap()
    xT_d = dram("xT", [128, NKC, S])
    posb_d = dram("posb", [128, S], I32)
    pT_d = dram("pT", [128, 2, OWN])
    win_d = dram("w_in_r", [48, 128, 2048])
    wout_d = dram("w_out_r", [16, 128, 2048])
    wg_d = dram("wg_r", [NE * 4, 128, 2048])
    wu_d = dram("wu_r", [NE * 4, 128, 2048])
    wd_d = dram("wd_r", [NE * 4, 128, 2048])
    wr_d = dram("wr_r", [128, NKC, 36])
    wpg_d = dram("wpg_r", [16, 128, 2048])
    wpp_d = dram("wpp_r", [16, 128, 256])
    cvec_d = dram("cvec", [128, 80])
    lamv_d = dram("lamv", [128, 4, 64])
    rb_d = dram("rb", [128, 36])
    cmat_d = dram("cmat", [128, 14, 128])
    mb_d = dram("mbias", [128, 640])
    out_d = dram("outT", [128, NKC, OWN], F32, kind="ExternalOutput")
    dbg_d = None
    if dbg:
        dbg_d = dram("dbg", [128, NKC, OWN], F32, kind="ExternalOutput")

    CONV_E = (29, 30, 31)
    wgb_d = dram("wgb", [len(CONV_E) * 4, 128, 2048], BF16, kind="Internal")
    wub_d = dram("wub", [len(CONV_E) * 4, 128, 2048], BF16, kind="Internal")
    wdb_d = dram("wdb", [len(CONV_E) * 4, 128, 2048], BF16, kind="Internal")
    conv_list = []
    for ci_, e_ in enumerate(CONV_E):
        for (sd, dd) in ((wg_d, wgb_d), (wu_d, wub_d), (wd_d, wdb_d)):
            for q_ in range(4):
                conv_list.append((sd[e_ * 4 + q_], dd[ci_ * 4 + q_]))
    conv_tk = [Tk(f"cv{i}") for i in range(12)]
    conv_i = [0]

    def w_src(plain, conv, e, q):
        return conv[CONV_E.index(e) * 4 + q] if e in CONV_E else plain[e * 4 + q]

    P = Prog(nc)
    A = Arena(nc)

    def conv_some(k):
        for _ in range(k):
            if conv_i[0] >= len(conv_list):
                return
            src_, dst_ = conv_list[conv_i[0]]
            tk_ = conv_tk[conv_i[0] % len(conv_tk)]
            conv_i[0] += 1
            P.op("pool", lambda h, src_=src_, dst_=dst_: h.dma_start(out=dst_, in_=src_), writes=[tk_], dma=True)

    es = ExitStack()
    banks = [es.enter_context(nc.psum_tensor(f"bank{i}", [128, 512], F32)) for i in range(8)]
    bk = [Tk(f"bank{i}") for i in range(8)]

    cvec = A.alloc([128, 80], F32); t_cvec = Tk("cvec")
    lamv = A.alloc([128, 4, 64], F32); t_lamv = Tk("lamv")
    rb = A.alloc([128, 36], F32); t_rb = Tk("rb")
    cmf = A.alloc([128, 14, 128], F32); t_cmf = Tk("cmf")
    cmb = A.alloc([128, 3, 128], BF16); t_cmb = Tk("cmb")
    sm = A.alloc([128, 32], F32); t_sm = Tk("sm")
    lnab = A.alloc([128, 64], F32); t_lnab = Tk("lnab")
    identb, trib, onesb = cmb[:, 0, :], cmb[:, 1, :], cmb[:, 2, :]
    identf, onesf, iota1 = cmf[:, 0, :], cmf[:, 2, :], cmf[:, 11, :]
    C_INV64, C_INV128, C_SG64, C_SG128, C_PM, C_SUBLN = 0, 1, 2, 3, 4, 5
    C_G1, C_B1, C_G2, C_B2, C_ZETA, C_TWOPI = 6, 22, 38, 54, 70, 74
    SM_NLAM, SM_SUBS = 5, 6

    P.op("sync", lambda h: h.dma_start(out=cvec[:], in_=cvec_d), writes=[t_cvec], dma=True)
    P.op("sync", lambda h: h.dma_start(out=lamv[:], in_=lamv_d), writes=[t_lamv], dma=True)
    P.op("sync", lambda h: h.dma_start(out=rb[:], in_=rb_d), writes=[t_rb], dma=True)
    P.op("sync", lambda h: h.dma_start(out=cmf[:], in_=cmat_d), writes=[t_cmf], dma=True)
    P.op("pool", lambda h: h.dma_start(out=cmb[:], in_=cmat_d[:, 0:3, :]), writes=[t_cmb], dma=True)
    mbias = A.alloc([128, 640], BF16); t_mbias = Tk("mbias")
    P.op("pool", lambda h: h.dma_start(out=mbias[:], in_=mb_d), writes=[t_mbias], dma=True)
    negtri, pmneg = mbias[:, 0:128], mbias[:, 128:640]

    A.ptr = (A.ptr + 4095) // 4096 * 4096
    Z_OFF = A.ptr
    xT = A.alloc([128, NKC, S], BF16)
    X_OFF = (A.ptr + 63) // 64 * 64
    attnT = A.alloc([128, NKC, OWN], BF16)
    t_xT = [Tk(f"xT{i}") for i in range(4)]
    t_attn = [Tk(f"attn{i}") for i in range(NKC)]
    for i in range(4):
        P.op("pool", lambda h, i=i: h.dma_start(out=xT[:, :, i * 512:(i + 1) * 512], in_=xT_d[:, :, i * 512:(i + 1) * 512]),
             writes=[t_xT[i]], dma=True)
    PH_A = A.ptr

    def make_ring(offs, dss=None, tag="w"):
        tiles = []
        for i, off in enumerate(offs):
            a = A.at(off, [128, NKC, 128], BF16)
            b = A.at(off, [128, 4, 512], BF16)
            c = A.at(off, [128, 2048], BF16)
            tiles.append((a, b, c, Tk(f"{tag}{i}", ds=(dss[i] if dss else None))))
        return {"tiles": tiles, "i": 0}

    def load_w_q(Q, ring, src, view=0, n=None):
        a, b, c, tk = ring["tiles"][ring["i"] % len(ring["tiles"])]
        ring["i"] += 1
        dst = (a, b, c)[view]
        if n is None:
            Q.op("pool", lambda h: h.dma_start(out=c[:], in_=src), writes=[tk], dma=True)
        else:
            Q.op("pool", lambda h: h.dma_start(out=c[:, 0:n], in_=src), writes=[tk], dma=True)
        return dst, tk

    def load_w(ring, src, view=0, n=None):
        a, b, c, tk = ring["tiles"][ring["i"] % len(ring["tiles"])]
        ring["i"] += 1
        dst = (a, b, c)[view]
        if n is None:
            P.op("pool", lambda h: h.dma_start(out=c[:], in_=src), writes=[tk], dma=True)
        else:
            P.op("pool", lambda h: h.dma_start(out=c[:, 0:n], in_=src), writes=[tk], dma=True)
        return dst, tk

    ringA_offs = []
    for _ in range(6):
        A.alloc([128, 2048], BF16)
        ringA_offs.append(A.ptr - 4096)
    ringA = make_ring(ringA_offs, tag="wa")

    lamtmp = A.alloc([128, 2, 64], F32); t_lamtmp = Tk("lamtmp")
    P.op("dve", lambda h: h.tensor_tensor(out=lamtmp[:, 0, :], in0=lamv[:, 0, :], in1=lamv[:, 1, :], op=ALU.mult), [t_lamv], [t_lamtmp])
    P.op("dve", lambda h: h.tensor_tensor(out=lamtmp[:, 1, :], in0=lamv[:, 2, :], in1=lamv[:, 3, :], op=ALU.mult), [t_lamv], [t_lamtmp])
    P.op("dve", lambda h: h.reduce_sum(out=sm[:, 0:1], in_=lamtmp[:, 0, :], axis=AX.X), [t_lamtmp], [t_sm])
    P.op("dve", lambda h: h.reduce_sum(out=sm[:, 1:2], in_=lamtmp[:, 1, :], axis=AX.X), [t_lamtmp], [t_sm])
    P.op("act", lambda h: h.activation(out=sm[:, 2:4], in_=sm[:, 0:2], func=AF.Exp), [t_sm], [t_sm])
    P.op("dve", lambda h: h.tensor_tensor(out=sm[:, 4:5], in0=sm[:, 2:3], in1=sm[:, 3:4], op=ALU.subtract), [t_sm], [t_sm])
    P.op("dve", lambda h: h.tensor_scalar(out=sm[:, 5:6], in0=sm[:, 4:5], scalar1=LAM_INIT, scalar2=-1.0, op0=ALU.add, op1=ALU.mult), [t_sm], [t_sm])
    P.op("dve", lambda h: h.tensor_scalar(out=sm[:, 6:7], in0=cvec[:, C_SUBLN:C_SUBLN + 1], scalar1=1.0 - LAM_INIT, scalar2=None, op0=ALU.mult), [t_cvec, t_sm], [t_sm])
    P.op("dve", lambda h: h.tensor_scalar(out=lnab[:, 0:32], in0=cvec[:, C_G1:C_G1 + 32], scalar1=ALPHA, scalar2=None, op0=ALU.mult), [t_cvec], [t_lnab])

    tabC = A.alloc([128, S], F32); tabS = A.alloc([128, S], F32)
    t_tab = Tk("tab")
    PH_HEAD = A.ptr
    posf = A.alloc([128, S], F32); t_posf = Tk("posf")
    tu = A.alloc([128, S], F32); tv = A.alloc([128, S], F32); tki = A.alloc([128, S], I32)
    t_tu, t_tv, t_tki = Tk("tu"), Tk("tv"), Tk("tki")
    P.op("pool", lambda h: h.dma_start(out=posf[:], in_=posb_d), writes=[t_posf], dma=True)

    def make_tables(inv_col, sg_col):
        P.op("dve", lambda h: h.tensor_scalar(out=tu[:], in0=posf[:], scalar1=cvec[:, inv_col:inv_col + 1], scalar2=None, op0=ALU.mult),
             [t_posf, t_cvec], [t_tu])
        for dst, shift, sc in ((tabS, 0.0, sg_col), (tabC, 0.25, C_TWOPI)):
            P.op("dve", lambda h, shift=shift: h.tensor_scalar(out=tv[:], in0=tu[:], scalar1=1.0 / (2 * math.pi), scalar2=shift, op0=ALU.mult, op1=ALU.add),
                 [t_tu], [t_tv])
            P.op("dve", lambda h: h.tensor_copy(out=tki[:], in_=tv[:]), [t_tv], [t_tki])
            P.op("dve", lambda h: h.tensor_copy(out=tki[:].bitcast(F32), in_=tki[:]), [t_tki], [t_tki])
            P.op("dve", lambda h: h.tensor_tensor(out=tv[:], in0=tv[:], in1=tki[:].bitcast(F32), op=ALU.subtract), [t_tv, t_tki], [t_tv])
            P.op("act", lambda h, dst=dst, sc=sc: h.activation(out=dst[:], in_=tv[:], func=AF.Sin, scale=cvec[:, sc:sc + 1]),
                 [t_tv, t_cvec], [t_tab])

    make_tables(C_INV128, C_SG128)

    A.ptr = PH_HEAD
    A.alloc([128, 22528], BF16)
    HB_OFF = A.ptr - 45056
    t_hb = Tk("hbregion")
    def after_tables(tks):
        P.op("dve", lambda h: h.memset(sm[:, 31:32], 0.0), [t_tab, t_tv, t_tki, t_tu, t_posf], tks)

    rtmp = [A.alloc([128, 512], F32) for _ in range(2)]
    t_rtmp = [Tk(f"rtmp{i}") for i in range(2)]

    def rope(ps, t_ps, dst_ap, t_dst, c0, half):
        t1, t2 = rtmp[0], rtmp[1]
        P.op("dve", lambda h: h.tensor_tensor(out=t1[:], in0=ps[:], in1=tabC[:, c0:c0 + 512], op=ALU.mult), [t_ps, t_tab], [t_rtmp[0]])
        nblk = 128 // half
        for g in range(nblk):
            src = (g ^ 1) * half
            dstp = g * half
            P.op("dve", lambda h, src=src, dstp=dstp: h.tensor_tensor(
                out=t2[dstp:dstp + half, :], in0=ps[src:src + half, :], in1=tabS[src:src + half, c0:c0 + 512], op=ALU.mult),
                [t_ps, t_tab], [t_rtmp[1]])
        P.op("pool", lambda h: h.tensor_tensor(out=dst_ap, in0=t1[:], in1=t2[:], op=ALU.add), [t_rtmp[0], t_rtmp[1]], [t_dst])

    def proj_fm(w, t_w, tok0, bank):
        ti = tok0 // 512
        for kc in range(NKC):
            P.op("pe", lambda h, kc=kc: h.matmul(banks[bank][:], lhsT=w[:, kc, :], rhs=xT[:, kc, tok0:tok0 + 512], start=(kc == 0), stop=(kc == NKC - 1)),
                 [t_w, t_xT[ti]], [bk[bank]])

    def proj_tm(w, t_w, blk, bank, col0, ncols=128, wc0=0):
        ti = blk // 4
        for kc in range(NKC):
            P.op("pe", lambda h, kc=kc: h.matmul(banks[bank][:, col0:col0 + ncols], lhsT=xT[:, kc, blk * 128:(blk + 1) * 128],
                                                  rhs=w[:, kc, wc0:wc0 + ncols], start=(kc == 0), stop=(kc == NKC - 1)),
                 [t_w, t_xT[ti]], [bk[bank]])

    def hview(off, shape, dt):
        return A.at(HB_OFF + off, shape, dt)

    qrT = hview(0, [128, OWN], BF16)
    qx = hview(2048, [128, OWN], BF16)
    krT = hview(4096, [128, S], BF16)
    Kz = hview(8192, [128, 16, 128], BF16)
    Vr = hview(12288, [128, 16, 256], BF16)
    sg = hview(20480, [128, 2, OWN], F32)
    yT = hview(28672, [128, 2, OWN], F32)
    Rst2 = [hview(36864, [128, 256], F32), hview(37888, [128, 256], F32)]
    Rbf_all = hview(38912, [128, 8, 256], BF16)
    innb_all = hview(43008, [128, 8, 128], BF16)
    t_qrT, t_qx, t_krT, t_Kz, t_Vr, t_sg, t_yT, t_R, t_Rbf, t_innb = [Tk(n) for n in "qrT qx krT Kz Vr sg yT R Rbf innb".split()]
    t_Rb2 = Tk("R2")
    after_tables([t_qrT, t_qx, t_krT, t_Kz, t_Vr, t_sg, t_yT, t_R, t_Rb2, t_Rbf, t_innb])
    ntmp = [A.alloc([128, 512], F32) for _ in range(4)]
    t_ntmp = [Tk(f"ntmp{i}") for i in range(4)]
    nb16 = [A.alloc([128, 512], BF16) for _ in range(2)]
    t_nb16 = [Tk(f"nb16{i}") for i in range(2)]
    PH_A_END = A.ptr

    CD = [float((1.0 - 2.0 ** (-5.0 - h)) ** 128) for h in range(4)]
    pj = [0, 1]
    pji = [0]

    def nextpj():
        pji[0] += 1
        return pj[pji[0] % 2]

    for hh in range(4):
        wv0, t_wv0 = load_w(ringA, win_d[32 + 2 * hh])
        wv1, t_wv1 = load_w(ringA, win_d[33 + 2 * hh])
        wg0, t_wg0 = load_w(ringA, win_d[40 + 2 * hh])
        wg1, t_wg1 = load_w(ringA, win_d[41 + 2 * hh])
        wq, t_wq = load_w(ringA, win_d[24 + hh])
        wk, t_wk = load_w(ringA, win_d[28 + hh])
        for blk in range(16):
            if blk % 2 == 0:
                b = nextpj()
            c0 = (blk % 2) * 256
            proj_tm(wv0, t_wv0, blk, b, c0, 128)
            proj_tm(wv1, t_wv1, blk, b, c0 + 128, 128)
            if blk % 2 == 1:
                P.op("act", lambda h, b=b, blk=blk: h.activation(out=Vr[:, blk - 1:blk + 1, :], in_=banks[b][:].rearrange("p (a c) -> p a c", a=2), func=AF.Copy),
                     [bk[b]], [t_Vr])
        for gi, (wg_, t_wg_) in enumerate(((wg0, t_wg0), (wg1, t_wg1))):
            for tt in range(2):
                b = nextpj()
                proj_fm(wg_, t_wg_, tt * 512, b)
                P.op("act", lambda h, b=b, gi=gi, tt=tt: h.activation(out=sg[:, gi, tt * 512:(tt + 1) * 512], in_=banks[b][:], func=AF.Silu),
                     [bk[b]], [t_sg])
        for tt in range(2):
            b = nextpj()
            proj_fm(wq, t_wq, tt * 512, b)
            rope(banks[b], bk[b], qrT[:, tt * 512:(tt + 1) * 512], t_qrT, tt * 512, 64)
        for m in range(8):
            P.op("dve", lambda h, m=m, hh=hh: h.tensor_tensor(out=qx[:, m * 128:(m + 1) * 128], in0=qrT[:, m * 128:(m + 1) * 128], in1=cmf[:, 7 + hh, :], op=ALU.mult),
                 [t_qrT, t_cmf], [t_qx])
        for tt in range(4):
            b = nextpj()
            proj_fm(wk, t_wk, tt * 512, b)
            rope(banks[b], bk[b], krT[:, tt * 512:(tt + 1) * 512], t_krT, tt * 512, 64)
        for blk in range(16):
            if blk % 4 == 0:
                b = nextpj()
            q4 = blk % 4
            pb = banks[b][:].bitcast(BF16)
            P.op("pe", lambda h, blk=blk, pb=pb, q4=q4: h.transpose(pb[:, q4 * 128:(q4 + 1) * 128], krT[:, blk * 128:(blk + 1) * 128], identb),
                 [t_krT, t_cmb], [bk[b]])
            if q4 == 3:
                P.op("act", lambda h, blk=blk, pb=pb, hh=hh: h.activation(out=Kz[:, blk - 3:blk + 1, :], in_=pb[:, 0:512].rearrange("p (a c) -> p a c", a=4), func=AF.Copy,
                                                                 scale=cvec[:, C_ZETA + hh:C_ZETA + hh + 1]),
                     [bk[b], t_cvec], [t_Kz])
        t_R2 = [t_R, t_Rb2]
        P.op("dve", lambda h: h.memset(Rst2[0][:], 0.0), [], [t_R2[0]])
        for m in range(8):
            bI = 2 + m // 4
            P.op("pe", lambda h, m=m, bI=bI: h.matmul(banks[bI][:, (m % 4) * 128:(m % 4 + 1) * 128], lhsT=krT[:, m * 128:(m + 1) * 128], rhs=qrT[:, m * 128:(m + 1) * 128],
                                                      start=True, stop=True), [t_krT, t_qrT], [bk[bI]])
        for m in range(8):
            bI = 2 + m // 4
            P.op("dve", lambda h, m=m, bI=bI, hh=hh: h.tensor_tensor(out=innb_all[:, m, :], in0=banks[bI][:, (m % 4) * 128:(m % 4 + 1) * 128], in1=cmf[:, 3 + hh, :], op=ALU.mult),
                 [bk[bI], t_cmf], [t_innb])
        for v in range(15):
            blk = (v // 2) if v % 2 == 1 else 8 + v // 2
            bK = 4 + v % 4
            P.op("pe", lambda h, blk=blk, bK=bK: h.matmul(banks[bK][:, 0:256], lhsT=Kz[:, blk, :], rhs=Vr[:, blk, :], start=True, stop=True), [t_Kz, t_Vr], [bk[bK]])
            P.op("dve", lambda h, hh=hh, v=v, bK=bK: h.scalar_tensor_tensor(out=Rst2[(v + 1) % 2][:], in0=Rst2[v % 2][:], scalar=CD[hh], in1=banks[bK][:, 0:256], op0=ALU.mult, op1=ALU.add),
                 [t_R2[v % 2], bk[bK]], [t_R2[(v + 1) % 2]])
            if v % 2 == 0:
                P.op("act", lambda h, v=v: h.activation(out=Rbf_all[:, v // 2, :], in_=Rst2[(v + 1) % 2][:], func=AF.Copy), [t_R2[(v + 1) % 2]], [t_Rbf])
        for m in range(8):
            bY = m % 2
            for i in range(2):
                P.op("pe", lambda h, i=i, m=m, bY=bY: h.matmul(banks[bY][:, i * 128:(i + 1) * 128], lhsT=Vr[:, m, i * 128:(i + 1) * 128], rhs=innb_all[:, m, :], start=True, stop=False),
                     [t_Vr, t_innb], [bk[bY]])
                P.op("pe", lambda h, i=i, m=m, bY=bY: h.matmul(banks[bY][:, i * 128:(i + 1) * 128], lhsT=Rbf_all[:, m, i * 128:(i + 1) * 128], rhs=qx[:, m * 128:(m + 1) * 128], start=False, stop=True),
                     [t_Rbf, t_qx], [bk[bY]])
            P.op("act", lambda h, m=m, bY=bY: h.activation(out=yT[:, :, m * 128:(m + 1) * 128], in_=banks[bY][:, 0:256].rearrange("p (a c) -> p a c", a=2), func=AF.Copy),
                 [bk[bY]], [t_yT])
        for tt in range(2):
            cs = slice(tt * 512, (tt + 1) * 512)
            for i in range(2):
                P.op("act", lambda h, i=i, cs=cs: h.activation(out=nb16[0][:], in_=yT[:, i, cs], func=AF.Copy), [t_yT], [t_nb16[0]])
                P.op("act", lambda h, i=i, cs=cs: h.activation(out=nb16[1][:], in_=yT[:, i, cs], func=AF.Square), [t_yT], [t_nb16[1]])
                P.op("pe", lambda h, i=i: h.matmul(banks[5][:], lhsT=onesb, rhs=nb16[0][:], start=(i == 0), stop=(i == 1)), [t_cmb, t_nb16[0]], [bk[5]])
                P.op("pe", lambda h, i=i: h.matmul(banks[6][:], lhsT=onesb, rhs=nb16[1][:], start=(i == 0), stop=(i == 1)), [t_cmb, t_nb16[1]], [bk[6]])
            mean, msq, var, rstd = ntmp
            P.op("dve", lambda h: h.tensor_scalar(out=mean[:], in0=banks[5][:], scalar1=1.0 / 256, scalar2=None, op0=ALU.mult), [bk[5]], [t_ntmp[0]])
            P.op("dve", lambda h: h.tensor_tensor(out=msq[:], in0=mean[:], in1=mean[:], op=ALU.mult), [t_ntmp[0]], [t_ntmp[1]])
            P.op("dve", lambda h: h.scalar_tensor_tensor(out=var[:], in0=banks[6][:], scalar=1.0 / 256, in1=msq[:], op0=ALU.mult, op1=ALU.subtract),
                 [bk[6], t_ntmp[1]], [t_ntmp[2]])
            P.op("dve", lambda h: h.tensor_scalar(out=var[:], in0=var[:], scalar1=EPS, scalar2=None, op0=ALU.add), [t_ntmp[2]], [t_ntmp[2]])
            P.op("act", lambda h: h.activation(out=msq[:], in_=var[:], func=AF.Ln), [t_ntmp[2]], [t_ntmp[1]])
            P.op("act", lambda h: h.activation(out=rstd[:], in_=msq[:], func=AF.Exp, scale=-0.5), [t_ntmp[1]], [t_ntmp[3]])
            for i in range(2):
                P.op("dve", lambda h, i=i, cs=cs: h.tensor_tensor(out=var[:], in0=yT[:, i, cs], in1=mean[:], op=ALU.subtract), [t_yT, t_ntmp[0]], [t_ntmp[2]])
                P.op("dve", lambda h: h.tensor_tensor(out=var[:], in0=var[:], in1=rstd[:], op=ALU.mult), [t_ntmp[2], t_ntmp[3]], [t_ntmp[2]])
                ch = 8 + 2 * hh + i
                P.op("dve", lambda h, i=i, cs=cs, ch=ch: h.tensor_tensor(out=attnT[:, ch, cs], in0=var[:], in1=sg[:, i, cs], op=ALU.mult),
                     [t_ntmp[2], t_sg], [t_attn[ch]])

    make_tables_reads = [t_qrT, t_qx, t_krT, t_Kz, t_Vr, t_sg, t_yT, t_R, t_Rb2, t_Rbf, t_innb]
    A.ptr = PH_A_END
    for (tab_, sel_) in ((tabC, 12), (tabS, 13)):
        for tt in range(4):
            P.op("pe", lambda h, tab_=tab_, sel_=sel_, tt=tt: h.matmul(banks[4 + tt][:], lhsT=cmf[:, sel_, :], rhs=tab_[:, tt * 512:(tt + 1) * 512], start=True, stop=True),
                 [t_cmf, t_tab], [bk[4 + tt]])
        for tt in range(4):
            P.op("act", lambda h, tab_=tab_, tt=tt: h.activation(out=tab_[:, tt * 512:(tt + 1) * 512], in_=banks[4 + tt][:], func=AF.Copy), [bk[4 + tt]], [t_tab])

    QTb = [hview(0, [128, OWN], BF16), hview(2048, [128, OWN], BF16)]
    KTb = [hview(4096, [128, S], BF16), hview(8192, [128, S], BF16)]
    Vdb = [hview(12288, [128, 16, 128], BF16), hview(16384, [128, 16, 128], BF16)]
    Pt = [[hview(20480 + (br * 3 + i) * 1024, [128, 512], BF16) for i in range(3)] for br in range(2)]
    pr1 = hview(26624, [128, 512], F32)
    pr2 = hview(28672, [128, 512], F32)
    pa = hview(30720, [128, 512], F32)
    pb_ = hview(32768, [128, 512], F32)
    posq = hview(34816, [128, 512], BF16)
    t_QTb, t_KTb, t_Vdb = [Tk("QT0"), Tk("QT1")], [Tk("KT0"), Tk("KT1")], [Tk("Vd0"), Tk("Vd1")]
    t_QT, t_KT, t_Vd = t_QTb[0], t_KTb[0], t_Vdb[0]
    t_Pt = [[Tk(f"Pt{br}{i}") for i in range(3)] for br in range(2)]
    t_pr1, t_pr2, t_pa, t_pb, t_posq = [Tk(n) for n in "pr1 pr2 pa pb posq".split()]
    P.op("dve", lambda h: h.memset(sm[:, 30:31], 0.0), make_tables_reads + [t_posf, t_tu, t_tv, t_tki, t_tab],
         t_QTb + t_KTb + t_Vdb + [t_pr1, t_pr2, t_pa, t_pb, t_posq] + [t for r in t_Pt for t in r])

    def proj_steps(hh):
        d = hh % 2
        QT, KT, Vd = QTb[d], KTb[d], Vdb[d]
        hold = {}
        steps = []

        def s_load():
            hold["q"] = load_w(ringA, win_d[hh])
            hold["k"] = load_w(ringA, win_d[8 + hh])
            hold["v"] = load_w(ringA, win_d[16 + hh])
        steps.append(s_load)
        for g4 in range(4):
            def s_v(g4=g4):
                wv, t_wv = hold["v"]
                b = nextpj()
                for blk in range(4 * g4, 4 * g4 + 4):
                    proj_tm(wv, t_wv, blk, b, (blk % 4) * 128, 128)
                P.op("act", lambda h, b=b, g4=g4: h.activation(out=Vd[:, 4 * g4:4 * g4 + 4, :], in_=banks[b][:].rearrange("p (a c) -> p a c", a=4), func=AF.Copy),
                     [bk[b]], [t_Vdb[d]])
            steps.append(s_v)
        for tt in range(2):
            def s_q(tt=tt):
                wq, t_wq = hold["q"]
                b = nextpj()
                proj_fm(wq, t_wq, tt * 512, b)
                rope(banks[b], bk[b], QT[:, tt * 512:(tt + 1) * 512], t_QTb[d], tt * 512, 32)
            steps.append(s_q)
        for tt in range(4):
            def s_k(tt=tt):
                wk, t_wk = hold["k"]
                b = nextpj()
                proj_fm(wk, t_wk, tt * 512, b)
                rope(banks[b], bk[b], KT[:, tt * 512:(tt + 1) * 512], t_KTb[d], tt * 512, 32)
            steps.append(s_k)
        return steps

    def attn_steps(hh):
        d = hh % 2
        QT, KT, Vd = QTb[d], KTb[d], Vdb[d]
        t_QT, t_KT, t_Vd = t_QTb[d], t_KTb[d], t_Vdb[d]
        steps = []
        for qt in range(2):
            kbs = []
            for m in range(4 * qt + 4):
                kbs.append((0, m))
                kbs.append((1, m))
            st = {"pend": None, "first": True}

            def issue_av(pend, first, last):
                blk_, c0_, slot_ = pend
                for br in range(2):
                    P.op("pe", lambda h, br=br, blk_=blk_, c0_=c0_, slot_=slot_: h.matmul(
                        banks[4 + br][:, c0_:512], lhsT=Vd[:, blk_, :], rhs=Pt[br][slot_][:, c0_:512], start=first, stop=last),
                        [t_Vd, t_Pt[br][slot_]], [bk[4 + br]])
                    P.op("pe", lambda h, br=br, c0_=c0_, slot_=slot_: h.matmul(
                        banks[6 + br][:, c0_:512], lhsT=onesb, rhs=Pt[br][slot_][:, c0_:512], start=first, stop=last),
                        [t_cmb, t_Pt[br][slot_]], [bk[6 + br]])

            for i, (sec, m) in enumerate(kbs):
                def s_blk(i=i, sec=sec, m=m, qt=qt, st=st, issue_av=issue_av):
                    blk = m if sec == 0 else 8 + m
                    c0 = max(m - 4 * qt, 0) * 128
                    slot = i % 3
                    diag = (sec == 0 and m >= 4 * qt)
                    phantom = (sec == 1 and m == 0)
                    for br in range(2):
                        P.op("pe", lambda h, br=br, blk=blk, c0=c0, qt=qt, last=not (diag or phantom): h.matmul(
                            banks[2 + br][:, c0:512], lhsT=KT[br * 64:(br + 1) * 64, blk * 128:(blk + 1) * 128],
                            rhs=QT[br * 64:(br + 1) * 64, qt * 512 + c0:(qt + 1) * 512], start=True, stop=last),
                            [t_KT, t_QT], [bk[2 + br]])
                        if diag:
                            P.op("pe", lambda h, br=br, c0=c0: h.matmul(banks[2 + br][:, c0:c0 + 128], lhsT=identb, rhs=negtri, start=False, stop=True),
                                 [t_cmb, t_mbias], [bk[2 + br]])
                        if phantom:
                            P.op("pe", lambda h, br=br, c0=c0: h.matmul(banks[2 + br][:, c0:512], lhsT=identb, rhs=pmneg[:, c0:512], start=False, stop=True),
                                 [t_cmb, t_mbias], [bk[2 + br]])
                        P.op("act", lambda h, br=br, c0=c0, slot=slot: h.activation(out=Pt[br][slot][:, c0:512], in_=banks[2 + br][:, c0:512], func=AF.Exp, scale=0.125),
                             [bk[2 + br]], [t_Pt[br][slot]])
                    if st["pend"] is not None:
                        issue_av(st["pend"], st["first"], False)
                        st["first"] = False
                    st["pend"] = (blk, c0, slot)
                steps.append(s_blk)

            def s_post(qt=qt, st=st, issue_av=issue_av):
                issue_av(st["pend"], st["first"], True)
                cs = slice(qt * 512, (qt + 1) * 512)
                P.op("act", lambda h: h.activation(out=pr1[:], in_=banks[6][:], func=AF.Ln), [bk[6]], [t_pr1])
                P.op("act", lambda h: h.activation(out=pr2[:], in_=banks[7][:], func=AF.Ln), [bk[7]], [t_pr2])
                P.op("act", lambda h: h.activation(out=pa[:], in_=banks[4][:], func=AF.Copy), [bk[4]], [t_pa])
                P.op("act", lambda h: h.activation(out=pb_[:], in_=banks[5][:], func=AF.Copy), [bk[5]], [t_pb])
                P.op("act", lambda h: h.activation(out=pr1[:], in_=pr1[:], func=AF.Exp, scale=-1.0), [t_pr1], [t_pr1])
                P.op("act", lambda h: h.activation(out=pr2[:], in_=pr2[:], func=AF.Exp, scale=-1.0), [t_pr2], [t_pr2])
                P.op("dve", lambda h: h.tensor_tensor(out=pa[:], in0=pa[:], in1=pr1[:], op=ALU.mult), [t_pa, t_pr1], [t_pa])
                P.op("dve", lambda h: h.tensor_tensor(out=pb_[:], in0=pb_[:], in1=pr2[:], op=ALU.mult), [t_pb, t_pr2], [t_pb])
                P.op("dve", lambda h: h.scalar_tensor_tensor(out=pa[:], in0=pb_[:], scalar=sm[:, SM_NLAM:SM_NLAM + 1], in1=pa[:], op0=ALU.mult, op1=ALU.add),
                     [t_pb, t_pa, t_sm], [t_pa])
                P.op("act", lambda h: h.activation(out=posq[:], in_=pa[:], func=AF.Square), [t_pa], [t_posq])
                b = nextpj()
                P.op("pe", lambda h, b=b: h.matmul(banks[b][:], lhsT=onesb, rhs=posq[:], start=True, stop=True), [t_cmb, t_posq], [bk[b]])
                P.op("dve", lambda h, b=b: h.tensor_scalar(out=pr1[:], in0=banks[b][:], scalar1=1.0 / 128, scalar2=EPS, op0=ALU.mult, op1=ALU.add), [bk[b]], [t_pr1])
                P.op("act", lambda h: h.activation(out=pr2[:], in_=pr1[:], func=AF.Ln), [t_pr1], [t_pr2])
                P.op("act", lambda h: h.activation(out=pr1[:], in_=pr2[:], func=AF.Exp, scale=-0.5), [t_pr2], [t_pr1])
                P.op("dve", lambda h, hh=hh, cs=cs: h.scalar_tensor_tensor(out=attnT[:, hh, cs], in0=pa[:], scalar=sm[:, SM_SUBS:SM_SUBS + 1], in1=pr1[:], op0=ALU.mult, op1=ALU.mult),
                     [t_pa, t_pr1, t_sm], [t_attn[hh]])
            steps.append(s_post)
        return steps

    for s_ in proj_steps(0):
        s_()
    for hh in range(8):
        a_steps = attn_steps(hh)
        p_steps = proj_steps(hh + 1) if hh < 7 else []
        pi = 0
        for ai, a_ in enumerate(a_steps):
            a_()
            target = ((ai + 1) * len(p_steps)) // len(a_steps)
            while pi < target:
                p_steps[pi]()
                pi += 1

    if dbg == "attn":
        dstage = A.at(Z_OFF, [128, NKC, OWN], F32)
        t_ds = Tk("ds")
        P.op("act", lambda h: h.activation(out=dstage[:], in_=attnT[:], func=AF.Copy), t_attn + t_xT, [t_ds])
        o = P.op("sync", lambda h: h.dma_start(out=dbg_d, in_=dstage[:]), [t_ds], [Tk("dbgo")], dma=True)
        info = P.emit(final_wait_ops=[o.idx])
        print("ops", info)
        return nc

    A.ptr = PH_A
    zT = A.at(Z_OFF, [128, NKC, OWN], F32)
    x1b = attnT
    t_z = [[Tk(f"z{ot}_{tt}") for tt in range(2)] for ot in range(NKC)]
    t_zld = [Tk(f"zld{g}") for g in range(4)]
    t_x1b = [Tk("x1b0"), Tk("x1b1")]
    A.ptr = (A.ptr + 4095) // 4096 * 4096
    A.alloc([128, 8, 2048], BF16)
    X2_OFF = A.ptr - 32768
    x1tm = A.at(X2_OFF, [128, 8, 2048], BF16)
    wr = A.alloc([128, NKC, 36], BF16); t_wr = Tk("wr")
    L = A.alloc([128, 8, 36], F32); t_L = Tk("L")
    comb = A.alloc([128, 8, 32], F32); t_comb = Tk("comb")
    rs = A.alloc([128, 16], F32); t_rs = Tk("rs")
    rw = A.alloc([128, 4, 32], F32); t_rw = Tk("rw")
    mask = A.alloc([128, 8, 32], F32); t_mask = Tk("mask")
    maskb = A.alloc([128, 8, 32], BF16); t_maskb = Tk("maskb")
    rankm = A.alloc([128, 8, 32], F32); t_rankm = Tk("rankm")
    combhl = A.alloc([128, 8, 32, 2], BF16); t_combhl = Tk("combhl")
    ctmp = A.alloc([128, 8, 32], F32); t_ctmp = Tk("ctmp")
    flagf = A.alloc([128, 2], F32); t_flagf = Tk("flagf")
    flagi = A.alloc([128, 2], I32); t_flag = Tk("flagi")
    A.alloc([128, 8704], F32)
    SC = A.ptr - 34816
    lt = [A.at(SC + i * 2048, [128, 512], F32) for i in range(6)]
    t_lt = [Tk(f"lt{i}") for i in range(6)]
    lb = [A.at(SC + 12288 + i * 1024, [128, 512], BF16) for i in range(4)]
    t_lb = [Tk(f"lb{i}") for i in range(4)]
    et = [A.at(SC + 16384 + i * 2048, [128, 512], F32) for i in range(4)]
    t_et = [Tk(f"et{i}") for i in range(4)]
    pTb = A.at(SC + 24576, [128, 2, OWN], BF16); t_pTb = Tk("pTb")
    A.ptr = (A.ptr + 4095) // 4096 * 4096
    n_rc = (SB_END - A.ptr) // 4096
    rc_offs = [A.ptr + i * 4096 for i in range(n_rc)]
    x2_offs = [X2_OFF + i * 4096 for i in range(8)]
    x_offs = [X_OFF + i * 4096 for i in range(8)]
    ringM = make_ring(rc_offs + x2_offs, dss=[t[3].ds for t in ringA["tiles"]] + [None] * (n_rc + 2), tag="wm")
    sub_ds = [DS() for _ in range(n_rc + 8)]
    print("ring tiles: common", n_rc, "total", n_rc + 8)

    allA = t_QTb + t_KTb + t_Vdb + [t_pr1, t_pr2, t_pa, t_pb, t_posq, t_tab, t_posf, t_tu, t_tv, t_tki] + [t for r in t_Pt for t in r] + t_rtmp + t_ntmp + t_nb16 \
        + [t[3] for t in ringA["tiles"]] + [t_lamtmp]
    P.op("dve", lambda h: h.memset(sm[:, 29:30], 0.0), allA + t_xT,
         [t for r in t_z for t in r] + t_zld + [t[3] for t in ringM["tiles"]] + t_lt + t_lb + t_et
         + [t_wr, t_L, t_comb, t_rs, t_rw, t_mask, t_maskb, t_rankm, t_combhl, t_ctmp, t_flagf, t_flag, t_pTb])

    def ln_stat(src, t_src, ot, tt):
        cs = slice(tt * 512, (tt + 1) * 512)
        i2 = ot % 2
        bs, bq = 4 + 2 * tt, 5 + 2 * tt
        P.op("act", lambda h: h.activation(out=lb[i2][:], in_=src[:, ot, cs], func=AF.Copy), [t_src[ot][tt]], [t_lb[i2]])
        P.op("act", lambda h: h.activation(out=lb[2 + i2][:], in_=src[:, ot, cs], func=AF.Square), [t_src[ot][tt]], [t_lb[2 + i2]])
        P.op("pe", lambda h: h.matmul(banks[bs][:], lhsT=onesb, rhs=lb[i2][:], start=(ot == 0), stop=(ot == NKC - 1)), [t_cmb, t_lb[i2]], [bk[bs]])
        P.op("pe", lambda h: h.matmul(banks[bq][:], lhsT=onesb, rhs=lb[2 + i2][:], start=(ot == 0), stop=(ot == NKC - 1)), [t_cmb, t_lb[2 + i2]], [bk[bq]])

    def ln_apply(src, t_src, tt, gcol, bcol, dst_bf, t_dst_bf, acc=None):
        cs = slice(tt * 512, (tt + 1) * 512)
        bs, bq = 4 + 2 * tt, 5 + 2 * tt
        mean, msq, var, rstd = lt[0], lt[1], lt[2], lt[3]
        P.op("dve", lambda h: h.tensor_scalar(out=mean[:], in0=banks[bs][:], scalar1=1.0 / D, scalar2=None, op0=ALU.mult), [bk[bs]], [t_lt[0]])
        P.op("dve", lambda h: h.tensor_tensor(out=msq[:], in0=mean[:], in1=mean[:], op=ALU.mult), [t_lt[0]], [t_lt[1]])
        P.op("dve", lambda h: h.scalar_tensor_tensor(out=var[:], in0=banks[bq][:], scalar=1.0 / D, in1=msq[:], op0=ALU.mult, op1=ALU.subtract), [bk[bq], t_lt[1]], [t_lt[2]])
        P.op("dve", lambda h: h.tensor_scalar(out=var[:], in0=var[:], scalar1=EPS, scalar2=None, op0=ALU.add), [t_lt[2]], [t_lt[2]])
        P.op("act", lambda h: h.activation(out=msq[:], in_=var[:], func=AF.Ln), [t_lt[2]], [t_lt[1]])
        P.op("act", lambda h: h.activation(out=rstd[:], in_=msq[:], func=AF.Exp, scale=-0.5), [t_lt[1]], [t_lt[3]])
        for ot in range(NKC):
            P.op("dve", lambda h, ot=ot: h.tensor_tensor(out=src[:, ot, cs], in0=src[:, ot, cs], in1=mean[:], op=ALU.subtract), [t_src[ot][tt], t_lt[0]], [t_src[ot][tt]])
            P.op("dve", lambda h, ot=ot: h.tensor_tensor(out=src[:, ot, cs], in0=src[:, ot, cs], in1=rstd[:], op=ALU.mult), [t_src[ot][tt], t_lt[3]], [t_src[ot][tt]])
            P.op("act", lambda h, ot=ot: h.activation(out=dst_bf[:, ot, cs], in_=src[:, ot, cs], func=AF.Identity,
                                                      scale=cvec[:, gcol + ot:gcol + ot + 1], bias=cvec[:, bcol + ot:bcol + ot + 1]),
                 [t_src[ot][tt], t_cvec], [t_dst_bf[tt]])
            if acc:
                P.op("act", lambda h, ot=ot: h.activation(out=src[:, ot, cs], in_=src[:, ot, cs], func=AF.Identity,
                                                          scale=lnab[:, ot:ot + 1], bias=lnab[:, 16 + ot:16 + ot + 1]),
                     [t_src[ot][tt], t_lnab], [t_src[ot][tt]])
            else:
                P.op("act", lambda h, ot=ot: h.activation(out=src[:, ot, cs], in_=src[:, ot, cs], func=AF.Identity,
                                                          scale=cvec[:, gcol + ot:gcol + ot + 1], bias=cvec[:, bcol + ot:bcol + ot + 1]),
                     [t_src[ot][tt], t_cvec], [t_src[ot][tt]])

    for g in range(4):
        P.op("sync", lambda h, g=g: h.dma_start(out=zT[:, g * 4:(g + 1) * 4, :], in_=xT_d[:, g * 4:(g + 1) * 4, 0:OWN]), writes=[t_zld[g]], dma=True)
    for ot in range(NKC):
        wo, t_wo = load_w_q(P, ringM, wout_d[ot])
        conv_some(3)
        for tt in range(2):
            cs = slice(tt * 512, (tt + 1) * 512)
            b = nextpj()
            for kc in range(NKC):
                P.op("pe", lambda h, kc=kc, b=b, cs=cs, wo=wo: h.matmul(banks[b][:], lhsT=wo[:, kc, :], rhs=attnT[:, kc, cs], start=(kc == 0), stop=(kc == NKC - 1)),
                     [t_wo, t_attn[kc]], [bk[b]])
            P.op("dve", lambda h, ot=ot, cs=cs, b=b: h.scalar_tensor_tensor(out=zT[:, ot, cs], in0=zT[:, ot, cs], scalar=ALPHA, in1=banks[b][:], op0=ALU.mult, op1=ALU.add),
                 [t_zld[ot // 4], bk[b]], [t_z[ot][tt]])
    P.op("dve", lambda h: h.memset(sm[:, 28:29], 0.0), t_attn, t_x1b)
    for tt in range(2):
        for ot in range(NKC):
            ln_stat(zT, t_z, ot, tt)
        ln_apply(zT, t_z, tt, C_G1, C_B1, x1b, t_x1b, acc=True)
    yacc = zT
    t_y = t_z

    if dbg == "x1":
        o = P.op("sync", lambda h: h.dma_start(out=dbg_d, in_=yacc[:]), [t for r in t_y for t in r], [Tk("dbgo")], dma=True)
        print("ops", P.emit(final_wait_ops=[o.idx]))
        return nc

    N_PRE = min(n_rc, 8)
    pre_src = [wg_d[q] for q in range(4)] + [wu_d[q] for q in range(4)]
    for i_ in range(N_PRE):
        a_, b_, c_, tk_ = ringM["tiles"][i_]
        P.op("pool", lambda h, c_=c_, i_=i_: h.dma_start(out=c_[:], in_=pre_src[i_]), writes=[tk_], dma=True)

    P.op("pool", lambda h: h.dma_start(out=wr[:], in_=wr_d), writes=[t_wr], dma=True)
    BIG = 1.0e30
    for tb in range(8):
        tt = tb // 4
        b = nextpj()
        for kc in range(NKC):
            P.op("pe", lambda h, kc=kc, b=b, tb=tb: h.matmul(banks[b][:, 0:36], lhsT=x1b[:, kc, tb * 128:(tb + 1) * 128], rhs=wr[:, kc, :], start=(kc == 0), stop=(kc == NKC - 1)),
                 [t_x1b[tt], t_wr], [bk[b]])
        Lt = L[:, tb, :]
        P.op("dve", lambda h, b=b, Lt=Lt: h.tensor_tensor(out=Lt, in0=banks[b][:, 0:36], in1=rb[:], op=ALU.add), [bk[b], t_rb], [t_L])
        gl = L[:, tb, 0:4]
        el = L[:, tb, 4:36]
        c = lambda i: rs[:, i:i + 1]
        P.op("dve", lambda h, gl=gl: h.reduce_max(out=c(0), in_=gl, axis=AX.X), [t_L], [t_rs])
        P.op("dve", lambda h, gl=gl: h.tensor_scalar(out=rw[:, 0, 0:4], in0=gl, scalar1=c(0), scalar2=None, op0=ALU.is_equal), [t_L, t_rs], [t_rw])
        P.op("dve", lambda h: h.tensor_scalar(out=c(1), in0=c(0), scalar1=-1.0, scalar2=None, op0=ALU.mult), [t_rs], [t_rs])
        P.op("act", lambda h, gl=gl: h.activation(out=rw[:, 0, 8:12], in_=gl, func=AF.Exp, bias=c(1)), [t_L, t_rs], [t_rw])
        P.op("dve", lambda h: h.reduce_sum(out=c(2), in_=rw[:, 0, 8:12], axis=AX.X), [t_rw], [t_rs])
        P.op("dve", lambda h: h.reciprocal(out=c(3), in_=c(2)), [t_rs], [t_rs])
        P.op("dve", lambda h: h.tensor_scalar(out=rw[:, 0, 4:8], in0=rw[:, 0, 0:4], scalar1=-1.0, scalar2=BIG, op0=ALU.add, op1=ALU.mult), [t_rw], [t_rw])
        for g in range(4):
            P.op("dve", lambda h, g=g, el=el: h.tensor_scalar(out=rw[:, 1, g * 8:(g + 1) * 8], in0=el[:, g * 8:(g + 1) * 8], scalar1=rw[:, 0, 4 + g:5 + g], scalar2=None, op0=ALU.add),
                 [t_L, t_rw], [t_rw])
        P.op("dve", lambda h: h.reduce_max(out=c(4), in_=rw[:, 1, :], axis=AX.X), [t_rw], [t_rs])
        P.op("dve", lambda h: h.tensor_scalar(out=rw[:, 2, :], in0=rw[:, 1, :], scalar1=c(4), scalar2=None, op0=ALU.is_equal), [t_rw, t_rs], [t_rw])
        P.op("dve", lambda h: h.scalar_tensor_tensor(out=rw[:, 1, :], in0=rw[:, 2, :], scalar=-BIG, in1=rw[:, 1, :], op0=ALU.mult, op1=ALU.add), [t_rw], [t_rw])
        P.op("dve", lambda h: h.reduce_max(out=c(5), in_=rw[:, 1, :], axis=AX.X), [t_rw], [t_rs])
        P.op("dve", lambda h: h.tensor_scalar(out=rw[:, 3, :], in0=rw[:, 1, :], scalar1=c(5), scalar2=None, op0=ALU.is_equal), [t_rw, t_rs], [t_rw])
        P.op("dve", lambda h, tb=tb: h.tensor_tensor(out=mask[:, tb, :], in0=rw[:, 2, :], in1=rw[:, 3, :], op=ALU.add), [t_rw], [t_mask])
        P.op("dve", lambda h: h.tensor_tensor(out=c(6), in0=c(5), in1=c(4), op=ALU.subtract), [t_rs], [t_rs])
        P.op("act", lambda h: h.activation(out=c(7), in_=c(6), func=AF.Exp), [t_rs], [t_rs])
        P.op("dve", lambda h: h.tensor_scalar(out=c(8), in0=c(7), scalar1=1.0, scalar2=None, op0=ALU.add), [t_rs], [t_rs])
        P.op("dve", lambda h: h.reciprocal(out=c(9), in_=c(8)), [t_rs], [t_rs])
        P.op("dve", lambda h: h.tensor_tensor(out=c(10), in0=c(9), in1=c(3), op=ALU.mult), [t_rs], [t_rs])
        P.op("dve", lambda h: h.tensor_tensor(out=c(11), in0=c(3), in1=c(10), op=ALU.subtract), [t_rs], [t_rs])
        P.op("dve", lambda h: h.tensor_scalar(out=rw[:, 2, :], in0=rw[:, 2, :], scalar1=c(10), scalar2=None, op0=ALU.mult), [t_rw, t_rs], [t_rw])
        P.op("dve", lambda h, tb=tb: h.scalar_tensor_tensor(out=comb[:, tb, :], in0=rw[:, 3, :], scalar=c(11), in1=rw[:, 2, :], op0=ALU.mult, op1=ALU.add),
             [t_rw, t_rs], [t_comb])

    CAP = 128
    P.op("dve", lambda h: h.tensor_copy(out=maskb[:], in_=mask[:]), [t_mask], [t_maskb])
    b = nextpj()
    for tb in range(8):
        P.op("pe", lambda h, tb=tb, b=b: h.matmul(banks[b][:, tb * 32:(tb + 1) * 32], lhsT=trib, rhs=maskb[:, tb, :], start=True, stop=(tb == 0)),
             [t_cmb, t_maskb], [bk[b]])
        for t2 in range(tb):
            P.op("pe", lambda h, tb=tb, t2=t2, b=b: h.matmul(banks[b][:, tb * 32:(tb + 1) * 32], lhsT=onesb, rhs=maskb[:, t2, :], start=False, stop=(t2 == tb - 1)),
                 [t_cmb, t_maskb], [bk[b]])
    P.op("dve", lambda h, b=b: h.tensor_tensor(out=rankm[:].rearrange("p a b -> p (a b)"), in0=banks[b][:, 0:256], in1=mask[:].rearrange("p a b -> p (a b)"), op=ALU.mult),
         [bk[b], t_mask], [t_rankm])
    b2 = nextpj()
    for tb in range(8):
        P.op("pe", lambda h, tb=tb, b2=b2: h.matmul(banks[b2][:, 0:32], lhsT=onesb, rhs=maskb[:, tb, :], start=(tb == 0), stop=(tb == 7)), [t_cmb, t_maskb], [bk[b2]])
    P.op("dve", lambda h, b2=b2: h.reduce_max(out=flagf[:, 0:1], in_=banks[b2][:, 0:32], axis=AX.X), [bk[b2]], [t_flagf])
    thr = -1.0 if force_dense else CAP + 0.5
    P.op("dve", lambda h: h.tensor_scalar(out=flagf[:, 1:2], in0=flagf[:, 0:1], scalar1=thr, scalar2=None, op0=ALU.is_gt), [t_flagf], [t_flagf])
    P.op("dve", lambda h: h.tensor_copy(out=flagi[:, 0:1], in_=flagf[:, 1:2]), [t_flagf], [t_flag])
    P.op("dve", lambda h: h.tensor_copy(out=combhl[:, :, :, 0], in_=comb[:]), [t_comb], [t_combhl])
    P.op("dve", lambda h: h.tensor_tensor(out=ctmp[:], in0=comb[:], in1=combhl[:, :, :, 0], op=ALU.subtract), [t_comb, t_combhl], [t_ctmp])
    P.op("dve", lambda h: h.tensor_copy(out=combhl[:, :, :, 1], in_=ctmp[:]), [t_ctmp], [t_combhl])

    def build_sub(Q, sparse):
        T = {}
        sbk = [Tk(f"sbank{i}") for i in range(8)]
        ring = make_ring(rc_offs + (x_offs if sparse else x2_offs), dss=sub_ds, tag="ws")
        s_y = [[Tk(f"sy{ot}_{tt}") for tt in range(2)] for ot in range(NKC)]
        s_x1b, s_x1tm, s_c = Tk("s_x1b"), Tk("s_x1tm"), Tk("s_const")
        xg = [A.at(SC + i * 4096, [128, NKC, 128], BF16) for i in range(2)]
        Sel = [A.at(SC + 8192 + i * 2048, [128, 8, 128], BF16) for i in range(2)]
        SelT = [A.at(SC + 12288 + i * 2048, [128, OWN], BF16) for i in range(2)]
        su = A.at(SC + 16384, [128, 512], F32)
        hh = A.at(SC + 18432, [128, 512], F32)
        hbf = A.at(SC + 20480, [128, 512], BF16)
        hT = [A.at(SC + 21504 + i * 1024, [128, 4, 128], BF16) for i in range(2)]
        yg = [A.at(SC + 23552 + i * 4096, [128, 2048], BF16) for i in range(2)]
        cs = [A.at(SC + 31744 + i * 64, [128, 2], F32) for i in range(2)]
        t_xg, t_Sel, t_SelT, t_hT, t_yg, t_cs = [[Tk(f"{n}{i}") for i in range(2)] for n in ("xg", "Sel", "SelT", "hT", "yg", "cs")]
        t_su, t_hh, t_hbf = Tk("su"), Tk("hh"), Tk("hbf")
        rot = [0]
        tbv = banks[4][:].bitcast(BF16)

        def stage_B(k, xg_of, t_xgk, cs_ap, t_csk, wts):
            for (wl, bank) in ((wts[0], 2), (wts[1], 3)):
                for kc in range(NKC):
                    wv, t_w = wl[kc // 4]
                    Q.op("pe", lambda h, kc=kc, wv=wv, bank=bank: h.matmul(banks[bank][:], lhsT=xg_of(kc), rhs=wv[:, kc % 4, :], start=(kc == 0), stop=(kc == NKC - 1)),
                         [t_xgk, t_w], [sbk[bank]])
            Q.op("act", lambda h: h.activation(out=su[:], in_=banks[2][:], func=AF.Silu), [sbk[2]], [t_su])
            Q.op("dve", lambda h: h.tensor_tensor(out=su[:], in0=banks[3][:], in1=su[:], op=ALU.mult), [sbk[3], t_su], [t_su])
            Q.op("dve", lambda h: h.tensor_scalar(out=hbf[:], in0=su[:], scalar1=cs_ap, scalar2=None, op0=ALU.mult), [t_su, t_csk], [t_hbf])

        def stage_CD(k, wts):
            for fc in range(4):
                Q.op("pe", lambda h, fc=fc: h.transpose(tbv[:, fc * 128:(fc + 1) * 128], hbf[:, fc * 128:(fc + 1) * 128], identb), [t_hbf, s_c], [sbk[4]])
            Q.op("act", lambda h: h.activation(out=hT[k][:], in_=tbv[:, 0:512].rearrange("p (a c) -> p a c", a=4), func=AF.Copy), [sbk[4]], [t_hT[k]])
            for og in range(4):
                wv, t_w = wts[2][og]
                bank = 5 + rot[0] % 3
                rot[0] += 1
                for fc in range(4):
                    Q.op("pe", lambda h, fc=fc, wv=wv, bank=bank: h.matmul(banks[bank][:], lhsT=hT[k][:, fc, :], rhs=wv[:, fc, :], start=(fc == 0), stop=(fc == 3)),
                         [t_hT[k], t_w], [sbk[bank]])
                Q.op("act", lambda h, og=og, bank=bank: h.activation(out=yg[k][:, og * 512:(og + 1) * 512], in_=banks[bank][:], func=AF.Copy), [sbk[bank]], [t_yg[k]])

        def stage_E(k, scat):
            for ot in range(NKC):
                for (rhs_ap, t_rhs, tt, col0, n) in scat:
                    bank = 5 + rot[0] % 3
                    rot[0] += 1
                    Q.op("pe", lambda h, ot=ot, rhs_ap=rhs_ap, bank=bank, n=n: h.matmul(banks[bank][:, 0:n], lhsT=yg[k][:, ot * 128:(ot + 1) * 128], rhs=rhs_ap, start=True, stop=True),
                         [t_yg[k], t_rhs], [sbk[bank]])
                    Q.op("dve", lambda h, ot=ot, bank=bank, n=n, col0=col0: h.tensor_tensor(out=yacc[:, ot, col0:col0 + n], in0=yacc[:, ot, col0:col0 + n], in1=banks[bank][:, 0:n], op=ALU.add),
                         [s_y[ot][tt], sbk[bank]], [s_y[ot][tt]])

        def load_gu(e):
            if e == 0:
                tl = []
                for i_ in range(8):
                    if i_ < N_PRE:
                        a_, b_, c_, tk_ = ring["tiles"][ring["i"] % len(ring["tiles"])]
                        ring["i"] += 1
                        tl.append((b_, tk_))
                    else:
                        tl.append(load_w_q(Q, ring, (wg_d if i_ < 4 else wu_d)[i_ % 4], view=1))
                return [tl[0:4], tl[4:8], None]
            wg_t = [load_w_q(Q, ring, w_src(wg_d, wgb_d, e, q), view=1) for q in range(4)]
            wu_t = [load_w_q(Q, ring, w_src(wu_d, wub_d, e, q), view=1) for q in range(4)]
            return [wg_t, wu_t, None]

        def load_d(e, w):
            w[2] = [load_w_q(Q, ring, w_src(wd_d, wdb_d, e, q), view=1) for q in range(4)]

        if sparse:
            for tb in range(8):
                for kc in range(NKC):
                    bank = (kc // 8) % 2
                    pbv = banks[bank][:].bitcast(BF16)
                    Q.op("pe", lambda h, tb=tb, kc=kc, pbv=pbv: h.transpose(pbv[:, (kc % 8) * 128:(kc % 8 + 1) * 128], x1b[:, kc, tb * 128:(tb + 1) * 128], identb),
                         [s_x1b, s_c], [sbk[bank]])
                    if kc % 8 == 7:
                        Q.op("act", lambda h, tb=tb, kc=kc, pbv=pbv: h.activation(out=x1tm[:, tb, (kc - 7) * 128:(kc + 1) * 128], in_=pbv[:, 0:1024], func=AF.Copy),
                             [sbk[bank]], [s_x1tm])
            Q.op("dve", lambda h: h.memset(sm[:, 25:26], 0.0), [], [s_x1b] + [t[3] for t in ring["tiles"][n_rc:]])
            Sel3 = Sel + [A.at(SC + 31872, [128, 8, 128], BF16)]
            t_Sel3 = t_Sel + [Tk("Sel2")]
            SelT3 = SelT + [A.at(SC + 18432, [128, OWN], BF16)]
            t_SelT3 = t_SelT + [Tk("SelT2")]

            def stage_S(e):
                j3 = e % 3
                for tb in range(8):
                    Q.op("dve", lambda h, tb=tb, e=e, j3=j3: h.tensor_scalar(out=Sel3[j3][:, tb, :], in0=iota1, scalar1=rankm[:, tb, e:e + 1], scalar2=None, op0=ALU.is_equal),
                         [s_c], [t_Sel3[j3]])

            def stage_A(e):
                k = e % 2
                j3 = e % 3
                Sl, t_Sl = Sel3[j3], t_Sel3[j3]
                for tb in range(8):
                    Q.op("pe", lambda h, tb=tb, Sl=Sl: h.transpose(tbv[:, tb * 128:(tb + 1) * 128], Sl[:, tb, :], identb), [t_Sl, s_c], [sbk[4]])
                Q.op("act", lambda h, j3=j3: h.activation(out=SelT3[j3][:], in_=tbv[:, 0:1024], func=AF.Copy), [sbk[4]], [t_SelT3[j3]])
                for tb in range(8):
                    Q.op("pe", lambda h, tb=tb, e=e, Sl=Sl: h.matmul(banks[4][:, 0:2], lhsT=Sl[:, tb, :], rhs=combhl[:, tb, e, :], start=(tb == 0), stop=(tb == 7)),
                         [t_Sl, s_c], [sbk[4]])
                Q.op("dve", lambda h, k=k: h.reduce_sum(out=cs[k][:, 0:1], in_=banks[4][:, 0:2], axis=AX.X), [sbk[4]], [t_cs[k]])
                for q in range(4):
                    bank = q % 2
                    for kk in range(4):
                        kc = 4 * q + kk
                        for tb in range(8):
                            Q.op("pe", lambda h, tb=tb, kc=kc, kk=kk, bank=bank, Sl=Sl: h.matmul(banks[bank][:, kk * 128:(kk + 1) * 128], lhsT=x1tm[:, tb, kc * 128:(kc + 1) * 128],
                                                                                          rhs=Sl[:, tb, :], start=(tb == 0), stop=(tb == 7)),
                                 [s_x1tm, t_Sl], [sbk[bank]])
                    Q.op("act", lambda h, q=q, bank=bank, k=k: h.activation(out=xg[k][:, 4 * q:4 * q + 4, :], in_=banks[bank][:].rearrange("p (a c) -> p a c", a=4), func=AF.Copy),
                         [sbk[bank]], [t_xg[k]])

            def sB(e, w):
                k = e % 2
                stage_B(k, (lambda kc, k=k: xg[k][:, kc, :]), t_xg[k], cs[k][:, 0:1], t_cs[k], w)

            def sE2(e0, e1):
                for ot in range(NKC):
                    for tt in range(2):
                        bank = 5 + rot[0] % 3
                        rot[0] += 1
                        for n_, e_ in enumerate((e0, e1)):
                            k_, j_ = e_ % 2, e_ % 3
                            Q.op("pe", lambda h, ot=ot, tt=tt, bank=bank, k_=k_, j_=j_, n_=n_: h.matmul(
                                banks[bank][:], lhsT=yg[k_][:, ot * 128:(ot + 1) * 128], rhs=SelT3[j_][:, tt * 512:(tt + 1) * 512], start=(n_ == 0), stop=(n_ == 1)),
                                [t_yg[k_], t_SelT3[j_]], [sbk[bank]])
                        Q.op("dve", lambda h, ot=ot, tt=tt, bank=bank: h.tensor_tensor(out=yacc[:, ot, tt * 512:(tt + 1) * 512], in0=yacc[:, ot, tt * 512:(tt + 1) * 512],
                                                                                 in1=banks[bank][:], op=ALU.add),
                             [s_y[ot][tt], sbk[bank]], [s_y[ot][tt]])

            W = {}
            W[0] = load_gu(0)
            load_d(0, W[0])
            stage_S(0)
            stage_S(1)
            stage_A(0)
            sB(0, W[0])
            W[1] = load_gu(1)
            for i in range(NE):
                if i + 2 < NE:
                    stage_S(i + 2)
                if i + 1 < NE:
                    stage_A(i + 1)
                stage_CD(i % 2, W[i])
                if i + 1 < NE:
                    load_d(i + 1, W[i + 1])
                    sB(i + 1, W[i + 1])
                    if i + 2 < NE:
                        W[i + 2] = load_gu(i + 2)
                if i % 2 == 1:
                    sE2(i - 1, i)
        else:
            for e in range(NE):
                w = load_gu(e)
                load_d(e, w)
                for tb in range(8):
                    k = tb % 2
                    stage_B(k, (lambda kc, tb=tb: x1b[:, kc, tb * 128:(tb + 1) * 128]), s_x1b, comb[:, tb, e:e + 1], s_c, w)
                    stage_CD(k, w)
                    stage_E(k, [(identb, s_c, tb // 4, tb * 128, 128)])
        fin = Q.op("dve", lambda h: h.memset(sm[:, 24:25], 0.0), [t for r in s_y for t in r], [Tk("fin")], signal=True)
        return fin

    SP = Prog(nc)
    fin_sp = build_sub(SP, True)
    SP.prepare()
    DN = Prog(nc, esem=SP.esem)
    fin_dn = build_sub(DN, False)
    DN.prepare()

    conv_some(len(conv_list))
    ALL = conv_tk + bk + t_x1b + [t for r in t_y for t in r] + [t[3] for t in ringM["tiles"]] + t_lt + t_lb + t_et \
        + [t_comb, t_rankm, t_combhl, t_cmf, t_cmb, t_sm, t_pTb, t_mask, t_maskb]
    t_gate = Tk("gate")
    t_blk = {e: Tk(f"blk_{e}") for e in ("act", "dve", "pool", "pe")}
    P.op("dve", lambda h: h.memset(sm[:, 23:24], 0.0), ALL, ALL + [t_gate])

    def blockfn(e):
        def fn(h):
            with h.register(f"flg_{e}") as r:
                h.reg_load(r, flagi[0:1, 0:1])
                with h.If_ne(r, 0):
                    DN.run_engine(e, h)
                    h.wait_ge(fin_dn.ticket[0], fin_dn.ticket[1])
                with h.Else():
                    SP.run_engine(e, h)
                    h.wait_ge(fin_sp.ticket[0], fin_sp.ticket[1])
            return h.nop()
        return fn

    for e in ("pool", "pe", "act", "dve"):
        P.op(e, blockfn(e), [t_gate, t_flag], [t_blk[e]])
    P.op("dve", lambda h: h.memset(sm[:, 22:23], 0.0), list(t_blk.values()), ALL)

    if dbg == "z2":
        o = P.op("sync", lambda h: h.dma_start(out=dbg_d, in_=yacc[:]), [t for r in t_y for t in r], [Tk("dbgo")], dma=True)
        print("ops", P.emit(final_wait_ops=[o.idx]), "sub", {e: (len(SP.per_eng[e]), len(DN.per_eng[e])) for e in ENGS})
        return nc

    x2b = x1b
    t_x2b = [Tk("x2b0"), Tk("x2b1")]
    P.op("dve", lambda h: h.memset(sm[:, 27:28], 0.0), t_x1b, t_x2b)
    for tt in range(2):
        for ot in range(NKC):
            ln_stat(yacc, t_y, ot, tt)
    P.op("pool", lambda h: h.dma_start(out=pTb[:], in_=pT_d), writes=[t_pTb], dma=True)
    outs = []
    t_out = [Tk(f"out{i}") for i in range(8)]
    eti = [0]
    for tt in range(2):
        ln_apply(yacc, t_y, tt, C_G2, C_B2, x2b, t_x2b, acc=False)
    for tt in range(2):
        cs = slice(tt * 512, (tt + 1) * 512)
        for ot in range(NKC):
            wpg, t_wpg = load_w_q(P, ringM, wpg_d[ot])
            wpp, t_wpp = load_w_q(P, ringM, wpp_d[ot], view=0, n=256)
            bg = 0 + ot % 2
            bp = 2 + ot % 2
            for kc in range(NKC):
                P.op("pe", lambda h, kc=kc, bg=bg, cs=cs, wpg=wpg: h.matmul(banks[bg][:], lhsT=wpg[:, kc, :], rhs=x2b[:, kc, cs], start=(kc == 0), stop=(kc == NKC - 1)),
                     [t_wpg, t_x2b[tt]], [bk[bg]])
            for kc in range(2):
                P.op("pe", lambda h, kc=kc, bp=bp, cs=cs, wpp=wpp: h.matmul(banks[bp][:], lhsT=wpp[:, kc, :], rhs=pTb[:, kc, cs], start=(kc == 0), stop=(kc == 1)),
                     [t_wpp, t_pTb], [bk[bp]])
            k = eti[0] % 2
            eti[0] += 1
            s_, t_s = et[k], t_et[k]
            u_, t_u = et[2 + k], t_et[2 + k]
            P.op("act", lambda h, bg=bg, s_=s_: h.activation(out=s_[:], in_=banks[bg][:], func=AF.Sigmoid), [bk[bg]], [t_s])
            P.op("dve", lambda h, bp=bp, s_=s_, u_=u_: h.tensor_tensor(out=u_[:], in0=banks[bp][:], in1=s_[:], op=ALU.mult), [bk[bp], t_s], [t_u])
            P.op("dve", lambda h, ot=ot, cs=cs, u_=u_: h.tensor_tensor(out=yacc[:, ot, cs], in0=yacc[:, ot, cs], in1=u_[:], op=ALU.add), [t_y[ot][tt], t_u], [t_y[ot][tt]])
            if ot % 4 == 3:
                g = ot // 4
                o = P.op("sync", lambda h, g=g, cs=cs: h.dma_start(out=out_d[:, g * 4:(g + 1) * 4, cs], in_=yacc[:, g * 4:(g + 1) * 4, cs]),
                         [t_y[o_][tt] for o_ in range(g * 4, g * 4 + 4)], [t_out[tt * 4 + g]], dma=True)
                outs.append(o.idx)
    info = P.emit(final_wait_ops=outs)
    print("ops", info, "sub", {e: (len(SP.per_eng[e]), len(DN.per_eng[e])) for e in ENGS})
    return nc


def _tile_w(w, ncol):
    K, N = w.shape
    return np.ascontiguousarray(w.reshape(K // 128, 128, N // ncol, ncol).transpose(2, 1, 0, 3))


def _consts(j):
    p = np.arange(128)
    cv = np.zeros((128, 80), np.float32)
    cv[:, 0] = THETA ** (-(p % 32) / 32.0)
    cv[:, 1] = THETA ** (-(p % 64) / 64.0)
    sg64 = np.where((p // 32) % 2 == 0, 1.0, -1.0)
    sg128 = np.where((p // 64) % 2 == 0, 1.0, -1.0)
    cv[:, 2] = 2 * math.pi * sg64
    cv[:, 3] = 2 * math.pi * sg128
    cv[:, 4] = float(j)
    cv[:, 74] = 2 * math.pi
    lg = np.log(1.0 - 2.0 ** (-5.0 - np.arange(4, dtype=np.float64)))
    n = np.arange(128, dtype=np.float64)
    scale = 128.0 ** -0.5
    for h in range(4):
        cv[:, 70 + h] = scale * np.exp((127.0 - n) * lg[h])
    cm = np.zeros((128, 14, 128), np.float32)
    pp_ = np.arange(128)
    cm[2 * (pp_ % 32), 12, pp_] = 1.0
    cm[2 * (pp_ % 32), 13, pp_] = np.where((pp_ // 32) % 2 == 0, 1.0, -1.0)
    cm[:, 11, :] = np.arange(1, 129, dtype=np.float32)[None, :]
    cm[:, 0, :] = np.eye(128)
    cm[:, 1, :] = (n[None, :] >= n[:, None])
    cm[:, 2, :] = 1.0
    rel = n[None, :] - n[:, None]
    for h in range(4):
        cm[:, 3 + h, :] = np.where(rel >= 0, np.exp(rel * lg[h]), 0.0) * scale
        cm[:, 7 + h, :] = np.exp((n + 1.0) * lg[h])[None, :]
    return cv, cm


_NC_CACHE = {}


def _prep_shared(inp):
    f = lambda k: np.asarray(inp[k], np.float32)
    sh = {}
    sh["w_in_r"] = _tile_w(f("w_in")[0], 128).reshape(48, 128, 2048)
    sh["w_out_r"] = _tile_w(f("w_out")[0], 128).reshape(16, 128, 2048)
    wg = f("w_exp_gate")[0]
    wu = f("w_exp_up")[0]
    wd = f("w_exp_down")[0]
    sh["wg_r"] = np.ascontiguousarray(wg.reshape(NE, 4, 4, 128, 512).transpose(0, 1, 3, 2, 4)).reshape(NE * 4, 128, 2048)
    sh["wu_r"] = np.ascontiguousarray(wu.reshape(NE, 4, 4, 128, 512).transpose(0, 1, 3, 2, 4)).reshape(NE * 4, 128, 2048)
    sh["wd_r"] = np.ascontiguousarray(wd.reshape(NE, 4, 128, 4, 512).transpose(0, 3, 2, 1, 4)).reshape(NE * 4, 128, 2048)
    wr = np.concatenate([f("w_router_group")[0], f("w_router_expert")[0]], axis=1)
    sh["wr_r"] = np.ascontiguousarray(wr.reshape(16, 128, 36).transpose(1, 0, 2))
    sh["wpg_r"] = _tile_w(f("w_ple_gate")[0], 128).reshape(16, 128, 2048)
    sh["wpp_r"] = _tile_w(f("w_ple_proj")[0], 128).reshape(16, 128, 256)
    rbv = np.concatenate([f("b_router_group")[0], f("b_router_expert")[0]])
    sh["rb"] = np.ascontiguousarray(np.broadcast_to(rbv[None, :], (128, 36)))
    lv = np.stack([f("da_lambda_q1")[0], f("da_lambda_k1")[0], f("da_lambda_q2")[0], f("da_lambda_k2")[0]])
    sh["lamv"] = np.ascontiguousarray(np.broadcast_to(lv[None], (128, 4, 64)))
    return sh


def _core_maps(inp, dbg=None):
    x = np.asarray(inp["x"], np.float32)
    pp = np.asarray(inp["p"], np.float32)[0]
    pos = np.asarray(inp["positions"]).astype(np.int32)
    sh = _prep_shared(inp)
    vecs = {k: np.asarray(inp[k], np.float32)[0] for k in ("ln1_g", "ln1_b", "ln2_g", "ln2_b", "da_subln_w")}
    maps = []
    for c in range(8):
        b, j = c // 2, c % 2
        own_blocks = [2 * m + j for m in range(8)]
        oth_blocks = [2 * m + j - 1 for m in range(8)]
        xb = x[b].reshape(16, 128, D)
        pb = pos[b].reshape(16, 128)
        xs = np.zeros((16, 128, D), np.float32)
        ps = np.zeros((16, 128), np.int32)
        for m in range(8):
            xs[m] = xb[own_blocks[m]]
            ps[m] = pb[own_blocks[m]]
            if oth_blocks[m] >= 0:
                xs[8 + m] = xb[oth_blocks[m]]
                ps[8 + m] = pb[oth_blocks[m]]
        xs = xs.reshape(S, D)
        xT = np.ascontiguousarray(xs.T.reshape(16, 128, S).transpose(1, 0, 2))
        pown = pp[b].reshape(16, 128, 256)[own_blocks].reshape(OWN, 256)
        pT = np.ascontiguousarray(pown.T.reshape(2, 128, OWN).transpose(1, 0, 2))
        cv, cm = _consts(j)
        cv[:, 5] = vecs["da_subln_w"]
        cv[:, 6:22] = vecs["ln1_g"].reshape(16, 128).T
        cv[:, 22:38] = vecs["ln1_b"].reshape(16, 128).T
        cv[:, 38:54] = vecs["ln2_g"].reshape(16, 128).T
        cv[:, 54:70] = vecs["ln2_b"].reshape(16, 128).T
        m_ = dict(sh)
        m_["xT"] = xT
        m_["posb"] = np.ascontiguousarray(np.broadcast_to(ps.reshape(1, S), (128, S)))
        m_["pT"] = pT
        mb = np.zeros((128, 640), np.float32)
        kk = np.arange(128)
        mb[:, 0:128] = np.where(kk[None, :] < kk[:, None], -30000.0, 0.0)
        mb[:, 128:640] = -30000.0 * (1 - j)
        m_["mbias"] = mb
        m_["cvec"] = cv
        m_["cmat"] = cm
        maps.append(m_)
    return maps


def _assemble(res_list, key="outT"):
    out = np.zeros((NB, S, D), np.float32)
    for c in range(8):
        b, j = c // 2, c % 2
        oT = np.asarray(res_list[c][key])
        o = oT.transpose(2, 1, 0).reshape(OWN, D)
        for m in range(8):
            g = 2 * m + j
            out[b, g * 128:(g + 1) * 128] = o[m * 128:(m + 1) * 128]
    return out


def kernel(**inputs):
    if "nc" not in _NC_CACHE:
        _NC_CACHE["nc"] = build()
    nc = _NC_CACHE["nc"]
    maps = _core_maps(inputs)
    res = run_bass_kernel_spmd(nc, maps, core_ids=list(range(8)))
    return _assemble(res.results)
```

```python
import math
from contextlib import ExitStack

import numpy as np
import concourse.bass as bass
import concourse.mybir as mybir
from concourse.bass_utils import run_bass_kernel_spmd

F32 = mybir.dt.float32
BF16 = mybir.dt.bfloat16
I32 = mybir.dt.int32
AF = mybir.ActivationFunctionType
ALU = mybir.AluOpType
AX = mybir.AxisListType

D = 2048
S = 2048
NB = 4
NKC = 16
OWN = 1024
ALPHA = 2.0 ** 0.25
LAM_INIT = 0.8 - 0.6
EPS = 1e-5
THETA = 10000.0
NE = 32
ENGS = ("sync", "act", "dve", "pool", "pe")
STRICT_SAME_ENGINE = True
SB_BASE = 16512
SB_END = 16512 + 212800


class DS:
    __slots__ = ("sem",)

    def __init__(self):
        self.sem = None


class Tk:
    __slots__ = ("name", "w", "r", "ds")

    def __init__(self, name, ds=None):
        self.name = name
        self.w = None
        self.r = []
        self.ds = ds if ds is not None else DS()


class Op:
    __slots__ = ("eng", "fn", "deps", "is_dma", "tk", "signal", "ticket", "idx")


class Prog:
    def __init__(self, nc, esem=None):
        self.nc = nc
        self.ops = []
        self.esem = esem
        self.per_eng = None

    def op(self, eng, fn, reads=(), writes=(), dma=False, signal=False):
        o = Op()
        o.eng = eng
        o.fn = fn
        o.is_dma = dma
        o.idx = len(self.ops)
        o.signal = dma or signal
        o.ticket = None
        o.tk = None
        deps = {}
        for t in reads:
            if t.w is not None:
                deps[t.w] = True
        for t in writes:
            if t.w is not None:
                deps.setdefault(t.w, False)
            for r in t.r:
                deps.setdefault(r, False)
        deps.pop(o.idx, None)
        o.deps = deps
        for t in reads:
            t.r.append(o.idx)
        for t in writes:
            t.w = o.idx
            t.r = []
        if dma:
            o.tk = writes[0]
        self.ops.append(o)
        return o

    def prepare(self):
        nc = self.nc
        ops = self.ops
        for o in ops:
            for d, is_raw in o.deps.items():
                s = ops[d]
                if s.is_dma:
                    continue
                if s.eng == o.eng and not is_raw and (o.eng == "pe" or not STRICT_SAME_ENGINE):
                    continue
                s.signal = True
        if self.esem is None:
            self.esem = {e: nc.alloc_semaphore(name=f"s_{e}_{id(self) % 9973}") for e in ENGS}
        ecount = {e: 0 for e in ENGS}
        dcount = {}
        for o in ops:
            if o.is_dma:
                ds = o.tk.ds
                if ds.sem is None:
                    ds.sem = nc.alloc_semaphore(name=f"d_{id(ds) % 99991}")
                dcount[id(ds)] = dcount.get(id(ds), 0) + 16
                o.ticket = (ds.sem, dcount[id(ds)])
            elif o.signal:
                ecount[o.eng] += 1
                o.ticket = (self.esem[o.eng], ecount[o.eng])
        self.per_eng = {e: [o for o in ops if o.eng == e] for e in ENGS}

    def run_engine(self, e, h):
        ops = self.ops
        clock = {}
        for o in self.per_eng[e]:
            need = {}
            for d, is_raw in o.deps.items():
                s = ops[d]
                if s.eng == e and not s.is_dma and not is_raw and (e == "pe" or not STRICT_SAME_ENGINE):
                    continue
                sem, val = s.ticket
                k = id(sem)
                if clock.get(k, 0) >= val:
                    continue
                if k not in need or need[k][1] < val:
                    need[k] = (sem, val)
            for k, (sem, val) in need.items():
                h.wait_ge(sem, val)
                clock[k] = val
            ins = o.fn(h)
            if o.ticket is not None:
                ins.then_inc(o.ticket[0], 16 if o.is_dma else 1)

    def emit(self, final_wait_ops=()):
        nc = self.nc
        self.prepare()
        finals = [self.ops[i].ticket for i in final_wait_ops]
        P = self

        with nc.Block() as block:
            @block.sync
            def _(h):
                P.run_engine("sync", h)
                for sem, val in finals:
                    h.wait_ge(sem, val)

            @block.scalar
            def _(h):
                P.run_engine("act", h)

            @block.vector
            def _(h):
                P.run_engine("dve", h)

            @block.gpsimd
            def _(h):
                P.run_engine("pool", h)

            @block.tensor
            def _(h):
                P.run_engine("pe", h)
        return {e: len(self.per_eng[e]) for e in ENGS}


class Arena:
    def __init__(self, nc):
        self.nc = nc
        self.big = nc.alloc_sbuf_tensor("big", [128, (SB_END - SB_BASE) // 4], F32)
        self.ptr = SB_BASE

    def at(self, off, shape, dt):
        esz = 2 if dt == BF16 else 4
        n = 1
        for s in shape[1:]:
            n *= s
        nbytes = n * esz
        assert off % 4 == 0 and nbytes % 4 == 0
        assert off >= SB_BASE and off + nbytes <= SB_END, f"SBUF overflow {off}+{nbytes}"
        a = (off - SB_BASE) // 4
        v = self.big[:, a:a + nbytes // 4]
        if dt != F32:
            v = v.bitcast(dt)
        if len(shape) == 3:
            v = v.rearrange("p (a b) -> p a b", a=shape[1])
        elif len(shape) == 4:
            v = v.rearrange("p (a b c) -> p a b c", a=shape[1], b=shape[2])
        return v

    def alloc(self, shape, dt):
        esz = 2 if dt == BF16 else 4
        n = esz
        for s in shape[1:]:
            n *= s
        self.ptr = (self.ptr + 63) // 64 * 64
        off = self.ptr
        self.ptr += (n + 3) // 4 * 4
        return self.at(off, shape, dt)


def build(dbg=None, force_dense=False):
    nc = bass.Bass("TRN2", target_bir_lowering=False)
    dram = lambda n, shp, dt=F32, kind="ExternalInput": nc.dram_tensor(n, list(shp), dt, kind=kind).ap()
    xT_d = dram("xT", [128, NKC, S])
    posb_d = dram("posb", [128, S], I32)
    pT_d = dram("pT", [128, 2, OWN])
    win_d = dram("w_in_r", [48, 128, 2048])
    wout_d = dram("w_out_r", [16, 128, 2048])
    wg_d = dram("wg_r", [NE * 4, 128, 2048])
    wu_d = dram("wu_r", [NE * 4, 128, 2048])
    wd_d = dram("wd_r", [NE * 4, 128, 2048])
    wr_d = dram("wr_r", [128, NKC, 36])
    wpg_d = dram("wpg_r", [16, 128, 2048])
    wpp_d = dram("wpp_r", [16, 128, 256])
    cvec_d = dram("cvec", [128, 80])
    lamv_d = dram("lamv", [128, 4, 64])
    rb_d = dram("rb", [128, 36])
    cmat_d = dram("cmat", [128, 14, 128])
    mb_d = dram("mbias", [128, 640])
    out_d = dram("outT", [128, NKC, OWN], F32, kind="ExternalOutput")
    dbg_d = None
    if dbg:
        dbg_d = dram("dbg", [128, NKC, OWN], F32, kind="ExternalOutput")

    P = Prog(nc)
    A = Arena(nc)
    es = ExitStack()
    banks = [es.enter_context(nc.psum_tensor(f"bank{i}", [128, 512], F32)) for i in range(8)]
    bk = [Tk(f"bank{i}") for i in range(8)]

    cvec = A.alloc([128, 80], F32); t_cvec = Tk("cvec")
    lamv = A.alloc([128, 4, 64], F32); t_lamv = Tk("lamv")
    rb = A.alloc([128, 36], F32); t_rb = Tk("rb")
    cmf = A.alloc([128, 14, 128], F32); t_cmf = Tk("cmf")
    cmb = A.alloc([128, 3, 128], BF16); t_cmb = Tk("cmb")
    sm = A.alloc([128, 32], F32); t_sm = Tk("sm")
    lnab = A.alloc([128, 64], F32); t_lnab = Tk("lnab")
    identb, trib, onesb = cmb[:, 0, :], cmb[:, 1, :], cmb[:, 2, :]
    identf, onesf, iota1 = cmf[:, 0, :], cmf[:, 2, :], cmf[:, 11, :]
    C_INV64, C_INV128, C_SG64, C_SG128, C_PM, C_SUBLN = 0, 1, 2, 3, 4, 5
    C_G1, C_B1, C_G2, C_B2, C_ZETA, C_TWOPI = 6, 22, 38, 54, 70, 74
    SM_NLAM, SM_SUBS = 5, 6

    P.op("sync", lambda h: h.dma_start(out=cvec[:], in_=cvec_d), writes=[t_cvec], dma=True)
    P.op("sync", lambda h: h.dma_start(out=lamv[:], in_=lamv_d), writes=[t_lamv], dma=True)
    P.op("sync", lambda h: h.dma_start(out=rb[:], in_=rb_d), writes=[t_rb], dma=True)
    P.op("sync", lambda h: h.dma_start(out=cmf[:], in_=cmat_d), writes=[t_cmf], dma=True)
    P.op("pool", lambda h: h.dma_start(out=cmb[:], in_=cmat_d[:, 0:3, :]), writes=[t_cmb], dma=True)
    mbias = A.alloc([128, 640], BF16); t_mbias = Tk("mbias")
    P.op("pool", lambda h: h.dma_start(out=mbias[:], in_=mb_d), writes=[t_mbias], dma=True)
    negtri, pmneg = mbias[:, 0:128], mbias[:, 128:640]

    A.ptr = (A.ptr + 4095) // 4096 * 4096
    Z_OFF = A.ptr
    xT = A.alloc([128, NKC, S], BF16)
    X_OFF = (A.ptr + 63) // 64 * 64
    attnT = A.alloc([128, NKC, OWN], BF16)
    t_xT = [Tk(f"xT{i}") for i in range(4)]
    t_attn = [Tk(f"attn{i}") for i in range(NKC)]
    def load_xT(i):
        P.op("pool", lambda h, i=i: h.dma_start(out=xT[:, :, i * 512:(i + 1) * 512], in_=xT_d[:, :, i * 512:(i + 1) * 512]),
             writes=[t_xT[i]], dma=True)
    load_xT(0)
    PH_A = A.ptr

    def make_ring(offs, dss=None, tag="w"):
        tiles = []
        for i, off in enumerate(offs):
            a = A.at(off, [128, NKC, 128], BF16)
            b = A.at(off, [128, 4, 512], BF16)
            c = A.at(off, [128, 2048], BF16)
            tiles.append((a, b, c, Tk(f"{tag}{i}", ds=(dss[i] if dss else None))))
        return {"tiles": tiles, "i": 0}

    def load_w_q(Q, ring, src, view=0, n=None):
        a, b, c, tk = ring["tiles"][ring["i"] % len(ring["tiles"])]
        ring["i"] += 1
        dst = (a, b, c)[view]
        if n is None:
            Q.op("pool", lambda h: h.dma_start(out=c[:], in_=src), writes=[tk], dma=True)
        else:
            Q.op("pool", lambda h: h.dma_start(out=c[:, 0:n], in_=src), writes=[tk], dma=True)
        return dst, tk

    def load_w(ring, src, view=0, n=None):
        a, b, c, tk = ring["tiles"][ring["i"] % len(ring["tiles"])]
        ring["i"] += 1
        dst = (a, b, c)[view]
        if n is None:
            P.op("pool", lambda h: h.dma_start(out=c[:], in_=src), writes=[tk], dma=True)
        else:
            P.op("pool", lambda h: h.dma_start(out=c[:, 0:n], in_=src), writes=[tk], dma=True)
        return dst, tk

    ringA_offs = []
    for _ in range(6):
        A.alloc([128, 2048], BF16)
        ringA_offs.append(A.ptr - 4096)
    ringA = make_ring(ringA_offs, tag="wa")

    lamtmp = A.alloc([128, 2, 64], F32); t_lamtmp = Tk("lamtmp")
    P.op("dve", lambda h: h.tensor_tensor(out=lamtmp[:, 0, :], in0=lamv[:, 0, :], in1=lamv[:, 1, :], op=ALU.mult), [t_lamv], [t_lamtmp])
    P.op("dve", lambda h: h.tensor_tensor(out=lamtmp[:, 1, :], in0=lamv[:, 2, :], in1=lamv[:, 3, :], op=ALU.mult), [t_lamv], [t_lamtmp])
    P.op("dve", lambda h: h.reduce_sum(out=sm[:, 0:1], in_=lamtmp[:, 0, :], axis=AX.X), [t_lamtmp], [t_sm])
    P.op("dve", lambda h: h.reduce_sum(out=sm[:, 1:2], in_=lamtmp[:, 1, :], axis=AX.X), [t_lamtmp], [t_sm])
    P.op("act", lambda h: h.activation(out=sm[:, 2:4], in_=sm[:, 0:2], func=AF.Exp), [t_sm], [t_sm])
    P.op("dve", lambda h: h.tensor_tensor(out=sm[:, 4:5], in0=sm[:, 2:3], in1=sm[:, 3:4], op=ALU.subtract), [t_sm], [t_sm])
    P.op("dve", lambda h: h.tensor_scalar(out=sm[:, 5:6], in0=sm[:, 4:5], scalar1=LAM_INIT, scalar2=-1.0, op0=ALU.add, op1=ALU.mult), [t_sm], [t_sm])
    P.op("dve", lambda h: h.tensor_scalar(out=sm[:, 6:7], in0=cvec[:, C_SUBLN:C_SUBLN + 1], scalar1=1.0 - LAM_INIT, scalar2=None, op0=ALU.mult), [t_cvec, t_sm], [t_sm])
    P.op("dve", lambda h: h.tensor_scalar(out=lnab[:, 0:32], in0=cvec[:, C_G1:C_G1 + 32], scalar1=ALPHA, scalar2=None, op0=ALU.mult), [t_cvec], [t_lnab])

    tabC = A.alloc([128, S], F32); tabS = A.alloc([128, S], F32)
    t_tab = Tk("tab")
    PH_HEAD = A.ptr
    posf = A.alloc([128, S], F32); t_posf = Tk("posf")
    tu = A.alloc([128, S], F32); tv = A.alloc([128, S], F32); tki = A.alloc([128, S], I32)
    t_tu, t_tv, t_tki = Tk("tu"), Tk("tv"), Tk("tki")
    P.op("pool", lambda h: h.dma_start(out=posf[:], in_=posb_d), writes=[t_posf], dma=True)

    def make_tables(inv_col, sg_col):
        P.op("dve", lambda h: h.tensor_scalar(out=tu[:], in0=posf[:], scalar1=cvec[:, inv_col:inv_col + 1], scalar2=None, op0=ALU.mult),
             [t_posf, t_cvec], [t_tu])
        for dst, shift, sc in ((tabS, 0.0, sg_col), (tabC, 0.25, C_TWOPI)):
            P.op("dve", lambda h, shift=shift: h.tensor_scalar(out=tv[:], in0=tu[:], scalar1=1.0 / (2 * math.pi), scalar2=shift, op0=ALU.mult, op1=ALU.add),
                 [t_tu], [t_tv])
            P.op("dve", lambda h: h.tensor_copy(out=tki[:], in_=tv[:]), [t_tv], [t_tki])
            P.op("dve", lambda h: h.tensor_copy(out=tki[:].bitcast(F32), in_=tki[:]), [t_tki], [t_tki])
            P.op("dve", lambda h: h.tensor_tensor(out=tv[:], in0=tv[:], in1=tki[:].bitcast(F32), op=ALU.subtract), [t_tv, t_tki], [t_tv])
            P.op("act", lambda h, dst=dst, sc=sc: h.activation(out=dst[:], in_=tv[:], func=AF.Sin, scale=cvec[:, sc:sc + 1]),
                 [t_tv, t_cvec], [t_tab])

    make_tables(C_INV128, C_SG128)

    A.ptr = PH_HEAD
    A.alloc([128, 22528], BF16)
    HB_OFF = A.ptr - 45056
    t_hb = Tk("hbregion")
    def after_tables(tks):
        P.op("dve", lambda h: h.memset(sm[:, 31:32], 0.0), [t_tab, t_tv, t_tki, t_tu, t_posf], tks)

    rtmp = [A.alloc([128, 512], F32) for _ in range(2)]
    t_rtmp = [Tk(f"rtmp{i}") for i in range(2)]

    def rope(ps, t_ps, dst_ap, t_dst, c0, half):
        t1, t2 = rtmp[0], rtmp[1]
        P.op("dve", lambda h: h.tensor_tensor(out=t1[:], in0=ps[:], in1=tabC[:, c0:c0 + 512], op=ALU.mult), [t_ps, t_tab], [t_rtmp[0]])
        nblk = 128 // half
        for g in range(nblk):
            src = (g ^ 1) * half
            dstp = g * half
            P.op("dve", lambda h, src=src, dstp=dstp: h.tensor_tensor(
                out=t2[dstp:dstp + half, :], in0=ps[src:src + half, :], in1=tabS[src:src + half, c0:c0 + 512], op=ALU.mult),
                [t_ps, t_tab], [t_rtmp[1]])
        P.op("pool", lambda h: h.tensor_tensor(out=dst_ap, in0=t1[:], in1=t2[:], op=ALU.add), [t_rtmp[0], t_rtmp[1]], [t_dst])

    def proj_fm(w, t_w, tok0, bank):
        ti = tok0 // 512
        for kc in range(NKC):
            P.op("pe", lambda h, kc=kc: h.matmul(banks[bank][:], lhsT=w[:, kc, :], rhs=xT[:, kc, tok0:tok0 + 512], start=(kc == 0), stop=(kc == NKC - 1)),
                 [t_w, t_xT[ti]], [bk[bank]])

    def proj_tm(w, t_w, blk, bank, col0, ncols=128, wc0=0):
        ti = blk // 4
        for kc in range(NKC):
            P.op("pe", lambda h, kc=kc: h.matmul(banks[bank][:, col0:col0 + ncols], lhsT=xT[:, kc, blk * 128:(blk + 1) * 128],
                                                  rhs=w[:, kc, wc0:wc0 + ncols], start=(kc == 0), stop=(kc == NKC - 1)),
                 [t_w, t_xT[ti]], [bk[bank]])

    def hview(off, shape, dt):
        return A.at(HB_OFF + off, shape, dt)

    qrT = hview(0, [128, OWN], BF16)
    qx = hview(2048, [128, OWN], BF16)
    krT = hview(4096, [128, S], BF16)
    Kz = hview(8192, [128, 16, 128], BF16)
    Vr = hview(12288, [128, 16, 256], BF16)
    sg = hview(20480, [128, 2, OWN], F32)
    yT = hview(28672, [128, 2, OWN], F32)
    Rst2 = [hview(36864, [128, 256], F32), hview(37888, [128, 256], F32)]
    Rbf_all = hview(38912, [128, 8, 256], BF16)
    innb_all = hview(43008, [128, 8, 128], BF16)
    t_qrT, t_qx, t_krT, t_Kz, t_Vr, t_sg, t_yT, t_R, t_Rbf, t_innb = [Tk(n) for n in "qrT qx krT Kz Vr sg yT R Rbf innb".split()]
    t_Rb2 = Tk("R2")
    after_tables([t_qrT, t_qx, t_krT, t_Kz, t_Vr, t_sg, t_yT, t_R, t_Rb2, t_Rbf, t_innb])
    ntmp = [A.alloc([128, 512], F32) for _ in range(4)]
    t_ntmp = [Tk(f"ntmp{i}") for i in range(4)]
    nb16 = [A.alloc([128, 512], BF16) for _ in range(2)]
    t_nb16 = [Tk(f"nb16{i}") for i in range(2)]
    PH_A_END = A.ptr

    CD = [float((1.0 - 2.0 ** (-5.0 - h)) ** 128) for h in range(4)]
    pj = [0, 1]
    pji = [0]

    def nextpj():
        pji[0] += 1
        return pj[pji[0] % 2]

    for hh in range(4):
        wv0, t_wv0 = load_w(ringA, win_d[32 + 2 * hh])
        wv1, t_wv1 = load_w(ringA, win_d[33 + 2 * hh])
        if hh == 0:
            for i_ in range(1, 4):
                load_xT(i_)
        wg0, t_wg0 = load_w(ringA, win_d[40 + 2 * hh])
        wg1, t_wg1 = load_w(ringA, win_d[41 + 2 * hh])
        wq, t_wq = load_w(ringA, win_d[24 + hh])
        wk, t_wk = load_w(ringA, win_d[28 + hh])
        for blk in range(16):
            if blk % 2 == 0:
                b = nextpj()
            c0 = (blk % 2) * 256
            proj_tm(wv0, t_wv0, blk, b, c0, 128)
            proj_tm(wv1, t_wv1, blk, b, c0 + 128, 128)
            if blk % 2 == 1:
                P.op("act", lambda h, b=b, blk=blk: h.activation(out=Vr[:, blk - 1:blk + 1, :], in_=banks[b][:].rearrange("p (a c) -> p a c", a=2), func=AF.Copy),
                     [bk[b]], [t_Vr])
        for gi, (wg_, t_wg_) in enumerate(((wg0, t_wg0), (wg1, t_wg1))):
            for tt in range(2):
                b = nextpj()
                proj_fm(wg_, t_wg_, tt * 512, b)
                P.op("act", lambda h, b=b, gi=gi, tt=tt: h.activation(out=sg[:, gi, tt * 512:(tt + 1) * 512], in_=banks[b][:], func=AF.Silu),
                     [bk[b]], [t_sg])
        for tt in range(2):
            b = nextpj()
            proj_fm(wq, t_wq, tt * 512, b)
            rope(banks[b], bk[b], qrT[:, tt * 512:(tt + 1) * 512], t_qrT, tt * 512, 64)
        for m in range(8):
            P.op("dve", lambda h, m=m, hh=hh: h.tensor_tensor(out=qx[:, m * 128:(m + 1) * 128], in0=qrT[:, m * 128:(m + 1) * 128], in1=cmf[:, 7 + hh, :], op=ALU.mult),
                 [t_qrT, t_cmf], [t_qx])
        for tt in range(4):
            b = nextpj()
            proj_fm(wk, t_wk, tt * 512, b)
            rope(banks[b], bk[b], krT[:, tt * 512:(tt + 1) * 512], t_krT, tt * 512, 64)
        for blk in range(16):
            if blk % 4 == 0:
                b = nextpj()
            q4 = blk % 4
            pb = banks[b][:].bitcast(BF16)
            P.op("pe", lambda h, blk=blk, pb=pb, q4=q4: h.transpose(pb[:, q4 * 128:(q4 + 1) * 128], krT[:, blk * 128:(blk + 1) * 128], identb),
                 [t_krT, t_cmb], [bk[b]])
            if q4 == 3:
                P.op("act", lambda h, blk=blk, pb=pb, hh=hh: h.activation(out=Kz[:, blk - 3:blk + 1, :], in_=pb[:, 0:512].rearrange("p (a c) -> p a c", a=4), func=AF.Copy,
                                                                 scale=cvec[:, C_ZETA + hh:C_ZETA + hh + 1]),
                     [bk[b], t_cvec], [t_Kz])
        t_R2 = [t_R, t_Rb2]
        P.op("dve", lambda h: h.memset(Rst2[0][:], 0.0), [], [t_R2[0]])
        for m in range(8):
            bI = 2 + m // 4
            P.op("pe", lambda h, m=m, bI=bI: h.matmul(banks[bI][:, (m % 4) * 128:(m % 4 + 1) * 128], lhsT=krT[:, m * 128:(m + 1) * 128], rhs=qrT[:, m * 128:(m + 1) * 128],
                                                      start=True, stop=True), [t_krT, t_qrT], [bk[bI]])
        for m in range(8):
            bI = 2 + m // 4
            P.op("dve", lambda h, m=m, bI=bI, hh=hh: h.tensor_tensor(out=innb_all[:, m, :], in0=banks[bI][:, (m % 4) * 128:(m % 4 + 1) * 128], in1=cmf[:, 3 + hh, :], op=ALU.mult),
                 [bk[bI], t_cmf], [t_innb])
        for v in range(15):
            blk = (v // 2) if v % 2 == 1 else 8 + v // 2
            bK = 4 + v % 4
            P.op("pe", lambda h, blk=blk, bK=bK: h.matmul(banks[bK][:, 0:256], lhsT=Kz[:, blk, :], rhs=Vr[:, blk, :], start=True, stop=True), [t_Kz, t_Vr], [bk[bK]])
            P.op("dve", lambda h, hh=hh, v=v, bK=bK: h.scalar_tensor_tensor(out=Rst2[(v + 1) % 2][:], in0=Rst2[v % 2][:], scalar=CD[hh], in1=banks[bK][:, 0:256], op0=ALU.mult, op1=ALU.add),
                 [t_R2[v % 2], bk[bK]], [t_R2[(v + 1) % 2]])
            if v % 2 == 0:
                P.op("act", lambda h, v=v: h.activation(out=Rbf_all[:, v // 2, :], in_=Rst2[(v + 1) % 2][:], func=AF.Copy), [t_R2[(v + 1) % 2]], [t_Rbf])
        for m in range(8):
            bY = m % 2
            for i in range(2):
                P.op("pe", lambda h, i=i, m=m, bY=bY: h.matmul(banks[bY][:, i * 128:(i + 1) * 128], lhsT=Vr[:, m, i * 128:(i + 1) * 128], rhs=innb_all[:, m, :], start=True, stop=False),
                     [t_Vr, t_innb], [bk[bY]])
                P.op("pe", lambda h, i=i, m=m, bY=bY: h.matmul(banks[bY][:, i * 128:(i + 1) * 128], lhsT=Rbf_all[:, m, i * 128:(i + 1) * 128], rhs=qx[:, m * 128:(m + 1) * 128], start=False, stop=True),
                     [t_Rbf, t_qx], [bk[bY]])
            P.op("act", lambda h, m=m, bY=bY: h.activation(out=yT[:, :, m * 128:(m + 1) * 128], in_=banks[bY][:, 0:256].rearrange("p (a c) -> p a c", a=2), func=AF.Copy),
                 [bk[bY]], [t_yT])
        for tt in range(2):
            cs = slice(tt * 512, (tt + 1) * 512)
            for i in range(2):
                P.op("act", lambda h, i=i, cs=cs: h.activation(out=nb16[0][:], in_=yT[:, i, cs], func=AF.Copy), [t_yT], [t_nb16[0]])
                P.op("act", lambda h, i=i, cs=cs: h.activation(out=nb16[1][:], in_=yT[:, i, cs], func=AF.Square), [t_yT], [t_nb16[1]])
                P.op("pe", lambda h, i=i: h.matmul(banks[5][:], lhsT=onesb, rhs=nb16[0][:], start=(i == 0), stop=(i == 1)), [t_cmb, t_nb16[0]], [bk[5]])
                P.op("pe", lambda h, i=i: h.matmul(banks[6][:], lhsT=onesb, rhs=nb16[1][:], start=(i == 0), stop=(i == 1)), [t_cmb, t_nb16[1]], [bk[6]])
            mean, msq, var, rstd = ntmp
            P.op("dve", lambda h: h.tensor_scalar(out=mean[:], in0=banks[5][:], scalar1=1.0 / 256, scalar2=None, op0=ALU.mult), [bk[5]], [t_ntmp[0]])
            P.op("dve", lambda h: h.tensor_tensor(out=msq[:], in0=mean[:], in1=mean[:], op=ALU.mult), [t_ntmp[0]], [t_ntmp[1]])
            P.op("dve", lambda h: h.scalar_tensor_tensor(out=var[:], in0=banks[6][:], scalar=1.0 / 256, in1=msq[:], op0=ALU.mult, op1=ALU.subtract),
                 [bk[6], t_ntmp[1]], [t_ntmp[2]])
            P.op("dve", lambda h: h.tensor_scalar(out=var[:], in0=var[:], scalar1=EPS, scalar2=None, op0=ALU.add), [t_ntmp[2]], [t_ntmp[2]])
            P.op("act", lambda h: h.activation(out=msq[:], in_=var[:], func=AF.Ln), [t_ntmp[2]], [t_ntmp[1]])
            P.op("act", lambda h: h.activation(out=rstd[:], in_=msq[:], func=AF.Exp, scale=-0.5), [t_ntmp[1]], [t_ntmp[3]])
            for i in range(2):
                P.op("dve", lambda h, i=i, cs=cs: h.tensor_tensor(out=var[:], in0=yT[:, i, cs], in1=mean[:], op=ALU.subtract), [t_yT, t_ntmp[0]], [t_ntmp[2]])
                P.op("dve", lambda h: h.tensor_tensor(out=var[:], in0=var[:], in1=rstd[:], op=ALU.mult), [t_ntmp[2], t_ntmp[3]], [t_ntmp[2]])
                ch = 8 + 2 * hh + i
                P.op("dve", lambda h, i=i, cs=cs, ch=ch: h.tensor_tensor(out=attnT[:, ch, cs], in0=var[:], in1=sg[:, i, cs], op=ALU.mult),
                     [t_ntmp[2], t_sg], [t_attn[ch]])

    make_tables_reads = [t_qrT, t_qx, t_krT, t_Kz, t_Vr, t_sg, t_yT, t_R, t_Rb2, t_Rbf, t_innb]
    A.ptr = PH_A_END
    for (tab_, sel_) in ((tabC, 12), (tabS, 13)):
        for tt in range(4):
            P.op("pe", lambda h, tab_=tab_, sel_=sel_, tt=tt: h.matmul(banks[4 + tt][:], lhsT=cmf[:, sel_, :], rhs=tab_[:, tt * 512:(tt + 1) * 512], start=True, stop=True),
                 [t_cmf, t_tab], [bk[4 + tt]])
        for tt in range(4):
            P.op("act", lambda h, tab_=tab_, tt=tt: h.activation(out=tab_[:, tt * 512:(tt + 1) * 512], in_=banks[4 + tt][:], func=AF.Copy), [bk[4 + tt]], [t_tab])

    QTb = [hview(0, [128, OWN], BF16), hview(2048, [128, OWN], BF16)]
    KTb = [hview(4096, [128, S], BF16), hview(8192, [128, S], BF16)]
    Vdb = [hview(12288, [128, 16, 128], BF16), hview(16384, [128, 16, 128], BF16)]
    Pt = [[hview(20480 + (br * 3 + i) * 1024, [128, 512], BF16) for i in range(3)] for br in range(2)]
    pr1 = hview(26624, [128, 512], F32)
    pr2 = hview(28672, [128, 512], F32)
    pa = hview(30720, [128, 512], F32)
    pb_ = hview(32768, [128, 512], F32)
    posq = hview(34816, [128, 512], BF16)
    t_QTb, t_KTb, t_Vdb = [Tk("QT0"), Tk("QT1")], [Tk("KT0"), Tk("KT1")], [Tk("Vd0"), Tk("Vd1")]
    t_QT, t_KT, t_Vd = t_QTb[0], t_KTb[0], t_Vdb[0]
    t_Pt = [[Tk(f"Pt{br}{i}") for i in range(3)] for br in range(2)]
    t_pr1, t_pr2, t_pa, t_pb, t_posq = [Tk(n) for n in "pr1 pr2 pa pb posq".split()]
    P.op("dve", lambda h: h.memset(sm[:, 30:31], 0.0), make_tables_reads + [t_posf, t_tu, t_tv, t_tki, t_tab],
         t_QTb + t_KTb + t_Vdb + [t_pr1, t_pr2, t_pa, t_pb, t_posq] + [t for r in t_Pt for t in r])

    def proj_steps(hh):
        d = hh % 2
        QT, KT, Vd = QTb[d], KTb[d], Vdb[d]
        hold = {}
        steps = []

        def s_load():
            hold["q"] = load_w(ringA, win_d[hh])
            hold["k"] = load_w(ringA, win_d[8 + hh])
            hold["v"] = load_w(ringA, win_d[16 + hh])
        steps.append(s_load)
        for g4 in range(4):
            def s_v(g4=g4):
                wv, t_wv = hold["v"]
                b = nextpj()
                for blk in range(4 * g4, 4 * g4 + 4):
                    proj_tm(wv, t_wv, blk, b, (blk % 4) * 128, 128)
                P.op("act", lambda h, b=b, g4=g4: h.activation(out=Vd[:, 4 * g4:4 * g4 + 4, :], in_=banks[b][:].rearrange("p (a c) -> p a c", a=4), func=AF.Copy),
                     [bk[b]], [t_Vdb[d]])
            steps.append(s_v)
        for tt in range(2):
            def s_q(tt=tt):
                wq, t_wq = hold["q"]
                b = nextpj()
                proj_fm(wq, t_wq, tt * 512, b)
                rope(banks[b], bk[b], QT[:, tt * 512:(tt + 1) * 512], t_QTb[d], tt * 512, 32)
            steps.append(s_q)
        for tt in range(4):
            def s_k(tt=tt):
                wk, t_wk = hold["k"]
                b = nextpj()
                proj_fm(wk, t_wk, tt * 512, b)
                rope(banks[b], bk[b], KT[:, tt * 512:(tt + 1) * 512], t_KTb[d], tt * 512, 32)
            steps.append(s_k)
        return steps

    def attn_steps(hh):
        d = hh % 2
        QT, KT, Vd = QTb[d], KTb[d], Vdb[d]
        t_QT, t_KT, t_Vd = t_QTb[d], t_KTb[d], t_Vdb[d]
        steps = []
        for qt in range(2):
            kbs = []
            for m in range(4 * qt + 4):
                kbs.append((0, m))
                kbs.append((1, m))
            st = {"pend": None, "first": True}

            def issue_av(pend, first, last):
                blk_, c0_, slot_ = pend
                for br in range(2):
                    P.op("pe", lambda h, br=br, blk_=blk_, c0_=c0_, slot_=slot_: h.matmul(
                        banks[4 + br][:, c0_:512], lhsT=Vd[:, blk_, :], rhs=Pt[br][slot_][:, c0_:512], start=first, stop=last),
                        [t_Vd, t_Pt[br][slot_]], [bk[4 + br]])
                    P.op("pe", lambda h, br=br, c0_=c0_, slot_=slot_: h.matmul(
                        banks[6 + br][:, c0_:512], lhsT=onesb, rhs=Pt[br][slot_][:, c0_:512], start=first, stop=last),
                        [t_cmb, t_Pt[br][slot_]], [bk[6 + br]])

            for i, (sec, m) in enumerate(kbs):
                def s_blk(i=i, sec=sec, m=m, qt=qt, st=st, issue_av=issue_av):
                    blk = m if sec == 0 else 8 + m
                    c0 = max(m - 4 * qt, 0) * 128
                    slot = i % 3
                    diag = (sec == 0 and m >= 4 * qt)
                    phantom = (sec == 1 and m == 0)
                    for br in range(2):
                        P.op("pe", lambda h, br=br, blk=blk, c0=c0, qt=qt, last=not (diag or phantom): h.matmul(
                            banks[2 + br][:, c0:512], lhsT=KT[br * 64:(br + 1) * 64, blk * 128:(blk + 1) * 128],
                            rhs=QT[br * 64:(br + 1) * 64, qt * 512 + c0:(qt + 1) * 512], start=True, stop=last),
                            [t_KT, t_QT], [bk[2 + br]])
                        if diag:
                            P.op("pe", lambda h, br=br, c0=c0: h.matmul(banks[2 + br][:, c0:c0 + 128], lhsT=identb, rhs=negtri, start=False, stop=True),
                                 [t_cmb, t_mbias], [bk[2 + br]])
                        if phantom:
                            P.op("pe", lambda h, br=br, c0=c0: h.matmul(banks[2 + br][:, c0:512], lhsT=identb, rhs=pmneg[:, c0:512], start=False, stop=True),
                                 [t_cmb, t_mbias], [bk[2 + br]])
                        P.op("act", lambda h, br=br, c0=c0, slot=slot: h.activation(out=Pt[br][slot][:, c0:512], in_=banks[2 + br][:, c0:512], func=AF.Exp, scale=0.125),
                             [bk[2 + br]], [t_Pt[br][slot]])
                    if st["pend"] is not None:
                        issue_av(st["pend"], st["first"], False)
                        st["first"] = False
                    st["pend"] = (blk, c0, slot)
                steps.append(s_blk)

            def s_post(qt=qt, st=st, issue_av=issue_av):
                issue_av(st["pend"], st["first"], True)
                cs = slice(qt * 512, (qt + 1) * 512)
                P.op("act", lambda h: h.activation(out=pr1[:], in_=banks[6][:], func=AF.Ln), [bk[6]], [t_pr1])
                P.op("act", lambda h: h.activation(out=pr2[:], in_=banks[7][:], func=AF.Ln), [bk[7]], [t_pr2])
                P.op("act", lambda h: h.activation(out=pa[:], in_=banks[4][:], func=AF.Copy), [bk[4]], [t_pa])
                P.op("act", lambda h: h.activation(out=pb_[:], in_=banks[5][:], func=AF.Copy), [bk[5]], [t_pb])
                P.op("act", lambda h: h.activation(out=pr1[:], in_=pr1[:], func=AF.Exp, scale=-1.0), [t_pr1], [t_pr1])
                P.op("act", lambda h: h.activation(out=pr2[:], in_=pr2[:], func=AF.Exp, scale=-1.0), [t_pr2], [t_pr2])
                P.op("dve", lambda h: h.tensor_tensor(out=pa[:], in0=pa[:], in1=pr1[:], op=ALU.mult), [t_pa, t_pr1], [t_pa])
                P.op("dve", lambda h: h.tensor_tensor(out=pb_[:], in0=pb_[:], in1=pr2[:], op=ALU.mult), [t_pb, t_pr2], [t_pb])
                P.op("dve", lambda h: h.scalar_tensor_tensor(out=pa[:], in0=pb_[:], scalar=sm[:, SM_NLAM:SM_NLAM + 1], in1=pa[:], op0=ALU.mult, op1=ALU.add),
                     [t_pb, t_pa, t_sm], [t_pa])
                P.op("act", lambda h: h.activation(out=posq[:], in_=pa[:], func=AF.Square), [t_pa], [t_posq])
                b = nextpj()
                P.op("pe", lambda h, b=b: h.matmul(banks[b][:], lhsT=onesb, rhs=posq[:], start=True, stop=True), [t_cmb, t_posq], [bk[b]])
                P.op("dve", lambda h, b=b: h.tensor_scalar(out=pr1[:], in0=banks[b][:], scalar1=1.0 / 128, scalar2=EPS, op0=ALU.mult, op1=ALU.add), [bk[b]], [t_pr1])
                P.op("act", lambda h: h.activation(out=pr2[:], in_=pr1[:], func=AF.Ln), [t_pr1], [t_pr2])
                P.op("act", lambda h: h.activation(out=pr1[:], in_=pr2[:], func=AF.Exp, scale=-0.5), [t_pr2], [t_pr1])
                P.op("dve", lambda h, hh=hh, cs=cs: h.scalar_tensor_tensor(out=attnT[:, hh, cs], in0=pa[:], scalar=sm[:, SM_SUBS:SM_SUBS + 1], in1=pr1[:], op0=ALU.mult, op1=ALU.mult),
                     [t_pa, t_pr1, t_sm], [t_attn[hh]])
            steps.append(s_post)
        return steps

    for s_ in proj_steps(0):
        s_()
    for hh in range(8):
        a_steps = attn_steps(hh)
        p_steps = proj_steps(hh + 1) if hh < 7 else []
        pi = 0
        for ai, a_ in enumerate(a_steps):
            a_()
            target = ((ai + 1) * len(p_steps)) // len(a_steps)
            while pi < target:
                p_steps[pi]()
                pi += 1

    if dbg == "attn":
        dstage = A.at(Z_OFF, [128, NKC, OWN], F32)
        t_ds = Tk("ds")
        P.op("act", lambda h: h.activation(out=dstage[:], in_=attnT[:], func=AF.Copy), t_attn + t_xT, [t_ds])
        o = P.op("sync", lambda h: h.dma_start(out=dbg_d, in_=dstage[:]), [t_ds], [Tk("dbgo")], dma=True)
        info = P.emit(final_wait_ops=[o.idx])
        print("ops", info)
        return nc

    A.ptr = PH_A
    zT = A.at(Z_OFF, [128, NKC, OWN], F32)
    x1b = attnT
    t_z = [[Tk(f"z{ot}_{tt}") for tt in range(2)] for ot in range(NKC)]
    t_zld = [Tk(f"zld{g}") for g in range(4)]
    t_x1b = [Tk("x1b0"), Tk("x1b1")]
    A.ptr = (A.ptr + 4095) // 4096 * 4096
    A.alloc([128, 8, 2048], BF16)
    X2_OFF = A.ptr - 32768
    x1tm = A.at(X2_OFF, [128, 8, 2048], BF16)
    wr = A.alloc([128, NKC, 36], BF16); t_wr = Tk("wr")
    L = A.alloc([128, 8, 36], F32); t_L = Tk("L")
    comb = A.alloc([128, 8, 32], F32); t_comb = Tk("comb")
    rs = A.alloc([128, 16], F32); t_rs = Tk("rs")
    rw = A.alloc([128, 4, 32], F32); t_rw = Tk("rw")
    mask = A.alloc([128, 8, 32], F32); t_mask = Tk("mask")
    maskb = A.alloc([128, 8, 32], BF16); t_maskb = Tk("maskb")
    rankm = A.alloc([128, 8, 32], F32); t_rankm = Tk("rankm")
    combhl = A.alloc([128, 8, 32, 2], BF16); t_combhl = Tk("combhl")
    ctmp = A.alloc([128, 8, 32], F32); t_ctmp = Tk("ctmp")
    flagf = A.alloc([128, 2], F32); t_flagf = Tk("flagf")
    flagi = A.alloc([128, 2], I32); t_flag = Tk("flagi")
    A.alloc([128, 8704], F32)
    SC = A.ptr - 34816
    lt = [A.at(SC + i * 2048, [128, 512], F32) for i in range(6)]
    t_lt = [Tk(f"lt{i}") for i in range(6)]
    lb = [A.at(SC + 12288 + i * 1024, [128, 512], BF16) for i in range(4)]
    t_lb = [Tk(f"lb{i}") for i in range(4)]
    et = [A.at(SC + 16384 + i * 2048, [128, 512], F32) for i in range(4)]
    t_et = [Tk(f"et{i}") for i in range(4)]
    pTb = A.at(SC + 24576, [128, 2, OWN], BF16); t_pTb = Tk("pTb")
    A.ptr = (A.ptr + 4095) // 4096 * 4096
    n_rc = (SB_END - A.ptr) // 4096
    rc_offs = [A.ptr + i * 4096 for i in range(n_rc)]
    x2_offs = [X2_OFF + i * 4096 for i in range(8)]
    x_offs = [X_OFF + i * 4096 for i in range(8)]
    ringM = make_ring(rc_offs + x2_offs, dss=[t[3].ds for t in ringA["tiles"]] + [None] * (n_rc + 2), tag="wm")
    sub_ds = [DS() for _ in range(n_rc + 8)]
    print("ring tiles: common", n_rc, "total", n_rc + 8)

    allA = t_QTb + t_KTb + t_Vdb + [t_pr1, t_pr2, t_pa, t_pb, t_posq, t_tab, t_posf, t_tu, t_tv, t_tki] + [t for r in t_Pt for t in r] + t_rtmp + t_ntmp + t_nb16 \
        + [t[3] for t in ringA["tiles"]] + [t_lamtmp]
    P.op("dve", lambda h: h.memset(sm[:, 29:30], 0.0), allA + t_xT,
         [t for r in t_z for t in r] + t_zld + [t[3] for t in ringM["tiles"]] + t_lt + t_lb + t_et
         + [t_wr, t_L, t_comb, t_rs, t_rw, t_mask, t_maskb, t_rankm, t_combhl, t_ctmp, t_flagf, t_flag, t_pTb])

    def ln_stat(src, t_src, ot, tt):
        cs = slice(tt * 512, (tt + 1) * 512)
        i2 = ot % 2
        bs, bq = 4 + 2 * tt, 5 + 2 * tt
        P.op("act", lambda h: h.activation(out=lb[i2][:], in_=src[:, ot, cs], func=AF.Copy), [t_src[ot][tt]], [t_lb[i2]])
        P.op("act", lambda h: h.activation(out=lb[2 + i2][:], in_=src[:, ot, cs], func=AF.Square), [t_src[ot][tt]], [t_lb[2 + i2]])
        P.op("pe", lambda h: h.matmul(banks[bs][:], lhsT=onesb, rhs=lb[i2][:], start=(ot == 0), stop=(ot == NKC - 1)), [t_cmb, t_lb[i2]], [bk[bs]])
        P.op("pe", lambda h: h.matmul(banks[bq][:], lhsT=onesb, rhs=lb[2 + i2][:], start=(ot == 0), stop=(ot == NKC - 1)), [t_cmb, t_lb[2 + i2]], [bk[bq]])

    def ln_apply(src, t_src, tt, gcol, bcol, dst_bf, t_dst_bf, acc=None):
        cs = slice(tt * 512, (tt + 1) * 512)
        bs, bq = 4 + 2 * tt, 5 + 2 * tt
        mean, msq, var, rstd = lt[0], lt[1], lt[2], lt[3]
        P.op("dve", lambda h: h.tensor_scalar(out=mean[:], in0=banks[bs][:], scalar1=1.0 / D, scalar2=None, op0=ALU.mult), [bk[bs]], [t_lt[0]])
        P.op("dve", lambda h: h.tensor_tensor(out=msq[:], in0=mean[:], in1=mean[:], op=ALU.mult), [t_lt[0]], [t_lt[1]])
        P.op("dve", lambda h: h.scalar_tensor_tensor(out=var[:], in0=banks[bq][:], scalar=1.0 / D, in1=msq[:], op0=ALU.mult, op1=ALU.subtract), [bk[bq], t_lt[1]], [t_lt[2]])
        P.op("dve", lambda h: h.tensor_scalar(out=var[:], in0=var[:], scalar1=EPS, scalar2=None, op0=ALU.add), [t_lt[2]], [t_lt[2]])
        P.op("act", lambda h: h.activation(out=msq[:], in_=var[:], func=AF.Ln), [t_lt[2]], [t_lt[1]])
        P.op("act", lambda h: h.activation(out=rstd[:], in_=msq[:], func=AF.Exp, scale=-0.5), [t_lt[1]], [t_lt[3]])
        for ot in range(NKC):
            P.op("dve", lambda h, ot=ot: h.tensor_tensor(out=src[:, ot, cs], in0=src[:, ot, cs], in1=mean[:], op=ALU.subtract), [t_src[ot][tt], t_lt[0]], [t_src[ot][tt]])
            P.op("dve", lambda h, ot=ot: h.tensor_tensor(out=src[:, ot, cs], in0=src[:, ot, cs], in1=rstd[:], op=ALU.mult), [t_src[ot][tt], t_lt[3]], [t_src[ot][tt]])
            P.op("act", lambda h, ot=ot: h.activation(out=dst_bf[:, ot, cs], in_=src[:, ot, cs], func=AF.Identity,
                                                      scale=cvec[:, gcol + ot:gcol + ot + 1], bias=cvec[:, bcol + ot:bcol + ot + 1]),
                 [t_src[ot][tt], t_cvec], [t_dst_bf[tt]])
            if acc:
                P.op("act", lambda h, ot=ot: h.activation(out=src[:, ot, cs], in_=src[:, ot, cs], func=AF.Identity,
                                                          scale=lnab[:, ot:ot + 1], bias=lnab[:, 16 + ot:16 + ot + 1]),
                     [t_src[ot][tt], t_lnab], [t_src[ot][tt]])
            else:
                P.op("act", lambda h, ot=ot: h.activation(out=src[:, ot, cs], in_=src[:, ot, cs], func=AF.Identity,
                                                          scale=cvec[:, gcol + ot:gcol + ot + 1], bias=cvec[:, bcol + ot:bcol + ot + 1]),
                     [t_src[ot][tt], t_cvec], [t_src[ot][tt]])

    for g in range(4):
        P.op("sync", lambda h, g=g: h.dma_start(out=zT[:, g * 4:(g + 1) * 4, :], in_=xT_d[:, g * 4:(g + 1) * 4, 0:OWN]), writes=[t_zld[g]], dma=True)
    for ot in range(NKC):
        wo, t_wo = load_w_q(P, ringM, wout_d[ot])
        for tt in range(2):
            cs = slice(tt * 512, (tt + 1) * 512)
            b = nextpj()
            for kc in range(NKC):
                P.op("pe", lambda h, kc=kc, b=b, cs=cs, wo=wo: h.matmul(banks[b][:], lhsT=wo[:, kc, :], rhs=attnT[:, kc, cs], start=(kc == 0), stop=(kc == NKC - 1)),
                     [t_wo, t_attn[kc]], [bk[b]])
            P.op("dve", lambda h, ot=ot, cs=cs, b=b: h.scalar_tensor_tensor(out=zT[:, ot, cs], in0=zT[:, ot, cs], scalar=ALPHA, in1=banks[b][:], op0=ALU.mult, op1=ALU.add),
                 [t_zld[ot // 4], bk[b]], [t_z[ot][tt]])
    P.op("dve", lambda h: h.memset(sm[:, 28:29], 0.0), t_attn, t_x1b)
    for tt in range(2):
        for ot in range(NKC):
            ln_stat(zT, t_z, ot, tt)
        ln_apply(zT, t_z, tt, C_G1, C_B1, x1b, t_x1b, acc=True)
    yacc = zT
    t_y = t_z

    if dbg == "x1":
        o = P.op("sync", lambda h: h.dma_start(out=dbg_d, in_=yacc[:]), [t for r in t_y for t in r], [Tk("dbgo")], dma=True)
        print("ops", P.emit(final_wait_ops=[o.idx]))
        return nc

    N_PRE = min(n_rc, 8)
    pre_src = [wg_d[q] for q in range(4)] + [wu_d[q] for q in range(4)]
    for i_ in range(N_PRE):
        a_, b_, c_, tk_ = ringM["tiles"][i_]
        P.op("pool", lambda h, c_=c_, i_=i_: h.dma_start(out=c_[:], in_=pre_src[i_]), writes=[tk_], dma=True)

    P.op("pool", lambda h: h.dma_start(out=wr[:], in_=wr_d), writes=[t_wr], dma=True)
    BIG = 1.0e30
    for tb in range(8):
        tt = tb // 4
        b = nextpj()
        for kc in range(NKC):
            P.op("pe", lambda h, kc=kc, b=b, tb=tb: h.matmul(banks[b][:, 0:36], lhsT=x1b[:, kc, tb * 128:(tb + 1) * 128], rhs=wr[:, kc, :], start=(kc == 0), stop=(kc == NKC - 1)),
                 [t_x1b[tt], t_wr], [bk[b]])
        Lt = L[:, tb, :]
        P.op("dve", lambda h, b=b, Lt=Lt: h.tensor_tensor(out=Lt, in0=banks[b][:, 0:36], in1=rb[:], op=ALU.add), [bk[b], t_rb], [t_L])
        gl = L[:, tb, 0:4]
        el = L[:, tb, 4:36]
        c = lambda i: rs[:, i:i + 1]
        P.op("dve", lambda h, gl=gl: h.reduce_max(out=c(0), in_=gl, axis=AX.X), [t_L], [t_rs])
        P.op("dve", lambda h, gl=gl: h.tensor_scalar(out=rw[:, 0, 0:4], in0=gl, scalar1=c(0), scalar2=None, op0=ALU.is_equal), [t_L, t_rs], [t_rw])
        P.op("dve", lambda h: h.tensor_scalar(out=c(1), in0=c(0), scalar1=-1.0, scalar2=None, op0=ALU.mult), [t_rs], [t_rs])
        P.op("act", lambda h, gl=gl: h.activation(out=rw[:, 0, 8:12], in_=gl, func=AF.Exp, bias=c(1)), [t_L, t_rs], [t_rw])
        P.op("dve", lambda h: h.reduce_sum(out=c(2), in_=rw[:, 0, 8:12], axis=AX.X), [t_rw], [t_rs])
        P.op("dve", lambda h: h.reciprocal(out=c(3), in_=c(2)), [t_rs], [t_rs])
        P.op("dve", lambda h: h.tensor_scalar(out=rw[:, 0, 4:8], in0=rw[:, 0, 0:4], scalar1=-1.0, scalar2=BIG, op0=ALU.add, op1=ALU.mult), [t_rw], [t_rw])
        for g in range(4):
            P.op("dve", lambda h, g=g, el=el: h.tensor_scalar(out=rw[:, 1, g * 8:(g + 1) * 8], in0=el[:, g * 8:(g + 1) * 8], scalar1=rw[:, 0, 4 + g:5 + g], scalar2=None, op0=ALU.add),
                 [t_L, t_rw], [t_rw])
        P.op("dve", lambda h: h.reduce_max(out=c(4), in_=rw[:, 1, :], axis=AX.X), [t_rw], [t_rs])
        P.op("dve", lambda h: h.tensor_scalar(out=rw[:, 2, :], in0=rw[:, 1, :], scalar1=c(4), scalar2=None, op0=ALU.is_equal), [t_rw, t_rs], [t_rw])
        P.op("dve", lambda h: h.scalar_tensor_tensor(out=rw[:, 1, :], in0=rw[:, 2, :], scalar=-BIG, in1=rw[:, 1, :], op0=ALU.mult, op1=ALU.add), [t_rw], [t_rw])
        P.op("dve", lambda h: h.reduce_max(out=c(5), in_=rw[:, 1, :], axis=AX.X), [t_rw], [t_rs])
        P.op("dve", lambda h: h.tensor_scalar(out=rw[:, 3, :], in0=rw[:, 1, :], scalar1=c(5), scalar2=None, op0=ALU.is_equal), [t_rw, t_rs], [t_rw])
        P.op("dve", lambda h, tb=tb: h.tensor_tensor(out=mask[:, tb, :], in0=rw[:, 2, :], in1=rw[:, 3, :], op=ALU.add), [t_rw], [t_mask])
        P.op("dve", lambda h: h.tensor_tensor(out=c(6), in0=c(5), in1=c(4), op=ALU.subtract), [t_rs], [t_rs])
        P.op("act", lambda h: h.activation(out=c(7), in_=c(6), func=AF.Exp), [t_rs], [t_rs])
        P.op("dve", lambda h: h.tensor_scalar(out=c(8), in0=c(7), scalar1=1.0, scalar2=None, op0=ALU.add), [t_rs], [t_rs])
        P.op("dve", lambda h: h.reciprocal(out=c(9), in_=c(8)), [t_rs], [t_rs])
        P.op("dve", lambda h: h.tensor_tensor(out=c(10), in0=c(9), in1=c(3), op=ALU.mult), [t_rs], [t_rs])
        P.op("dve", lambda h: h.tensor_tensor(out=c(11), in0=c(3), in1=c(10), op=ALU.subtract), [t_rs], [t_rs])
        P.op("dve", lambda h: h.tensor_scalar(out=rw[:, 2, :], in0=rw[:, 2, :], scalar1=c(10), scalar2=None, op0=ALU.mult), [t_rw, t_rs], [t_rw])
        P.op("dve", lambda h, tb=tb: h.scalar_tensor_tensor(out=comb[:, tb, :], in0=rw[:, 3, :], scalar=c(11), in1=rw[:, 2, :], op0=ALU.mult, op1=ALU.add),
             [t_rw, t_rs], [t_comb])

    x2_tks = [t[3] for t in ringM["tiles"][n_rc:]]
    for tb in range(8):
        for kc in range(NKC):
            bank = (kc // 8) % 2
            pbv = banks[bank][:].bitcast(BF16)
            P.op("pe", lambda h, tb=tb, kc=kc, pbv=pbv: h.transpose(pbv[:, (kc % 8) * 128:(kc % 8 + 1) * 128], x1b[:, kc, tb * 128:(tb + 1) * 128], identb),
                 [t_x1b[tb // 4], t_cmb], [bk[bank]])
            if kc % 8 == 7:
                P.op("act", lambda h, tb=tb, kc=kc, pbv=pbv: h.activation(out=x1tm[:, tb, (kc - 7) * 128:(kc + 1) * 128], in_=pbv[:, 0:1024], func=AF.Copy),
                     [bk[bank]], x2_tks)

    CAP = 128
    P.op("dve", lambda h: h.tensor_copy(out=maskb[:], in_=mask[:]), [t_mask], [t_maskb])
    b = nextpj()
    for tb in range(8):
        P.op("pe", lambda h, tb=tb, b=b: h.matmul(banks[b][:, tb * 32:(tb + 1) * 32], lhsT=trib, rhs=maskb[:, tb, :], start=True, stop=(tb == 0)),
             [t_cmb, t_maskb], [bk[b]])
        for t2 in range(tb):
            P.op("pe", lambda h, tb=tb, t2=t2, b=b: h.matmul(banks[b][:, tb * 32:(tb + 1) * 32], lhsT=onesb, rhs=maskb[:, t2, :], start=False, stop=(t2 == tb - 1)),
                 [t_cmb, t_maskb], [bk[b]])
    P.op("dve", lambda h, b=b: h.tensor_tensor(out=rankm[:].rearrange("p a b -> p (a b)"), in0=banks[b][:, 0:256], in1=mask[:].rearrange("p a b -> p (a b)"), op=ALU.mult),
         [bk[b], t_mask], [t_rankm])
    b2 = nextpj()
    for tb in range(8):
        P.op("pe", lambda h, tb=tb, b2=b2: h.matmul(banks[b2][:, 0:32], lhsT=onesb, rhs=maskb[:, tb, :], start=(tb == 0), stop=(tb == 7)), [t_cmb, t_maskb], [bk[b2]])
    P.op("dve", lambda h, b2=b2: h.reduce_max(out=flagf[:, 0:1], in_=banks[b2][:, 0:32], axis=AX.X), [bk[b2]], [t_flagf])
    thr = -1.0 if force_dense else CAP + 0.5
    P.op("dve", lambda h: h.tensor_scalar(out=flagf[:, 1:2], in0=flagf[:, 0:1], scalar1=thr, scalar2=None, op0=ALU.is_gt), [t_flagf], [t_flagf])
    P.op("dve", lambda h: h.tensor_copy(out=flagi[:, 0:1], in_=flagf[:, 1:2]), [t_flagf], [t_flag])
    P.op("dve", lambda h: h.tensor_copy(out=combhl[:, :, :, 0], in_=comb[:]), [t_comb], [t_combhl])
    P.op("dve", lambda h: h.tensor_tensor(out=ctmp[:], in0=comb[:], in1=combhl[:, :, :, 0], op=ALU.subtract), [t_comb, t_combhl], [t_ctmp])
    P.op("dve", lambda h: h.tensor_copy(out=combhl[:, :, :, 1], in_=ctmp[:]), [t_ctmp], [t_combhl])

    def build_sub(Q, sparse):
        T = {}
        sbk = [Tk(f"sbank{i}") for i in range(8)]
        ring = make_ring(rc_offs + (x_offs if sparse else x2_offs), dss=sub_ds, tag="ws")
        s_y = [[Tk(f"sy{ot}_{tt}") for tt in range(2)] for ot in range(NKC)]
        s_x1b, s_x1tm, s_c = Tk("s_x1b"), Tk("s_x1tm"), Tk("s_const")
        xg = [A.at(SC + i * 4096, [128, NKC, 128], BF16) for i in range(2)]
        Sel = [A.at(SC + 8192 + i * 2048, [128, 8, 128], BF16) for i in range(2)]
        SelT = [A.at(SC + 12288 + i * 2048, [128, OWN], BF16) for i in range(2)]
        su = A.at(SC + 16384, [128, 512], F32)
        hh = A.at(SC + 18432, [128, 512], F32)
        hbf = A.at(SC + 20480, [128, 512], BF16)
        hT = [A.at(SC + 21504 + i * 1024, [128, 4, 128], BF16) for i in range(2)]
        yg = [A.at(SC + 23552 + i * 4096, [128, 2048], BF16) for i in range(2)]
        cs = [A.at(SC + 31744 + i * 64, [128, 2], F32) for i in range(2)]
        t_xg, t_Sel, t_SelT, t_hT, t_yg, t_cs = [[Tk(f"{n}{i}") for i in range(2)] for n in ("xg", "Sel", "SelT", "hT", "yg", "cs")]
        t_su, t_hh, t_hbf = Tk("su"), Tk("hh"), Tk("hbf")
        rot = [0]
        tbv = banks[4][:].bitcast(BF16)

        def stage_B(k, xg_of, t_xgk, cs_ap, t_csk, wts):
            for (wl, bank) in ((wts[0], 2), (wts[1], 3)):
                for kc in range(NKC):
                    wv, t_w = wl[kc // 4]
                    Q.op("pe", lambda h, kc=kc, wv=wv, bank=bank: h.matmul(banks[bank][:], lhsT=xg_of(kc), rhs=wv[:, kc % 4, :], start=(kc == 0), stop=(kc == NKC - 1)),
                         [t_xgk, t_w], [sbk[bank]])
            Q.op("act", lambda h: h.activation(out=su[:], in_=banks[2][:], func=AF.Silu), [sbk[2]], [t_su])
            Q.op("dve", lambda h: h.tensor_tensor(out=su[:], in0=banks[3][:], in1=su[:], op=ALU.mult), [sbk[3], t_su], [t_su])
            Q.op("dve", lambda h: h.tensor_scalar(out=hbf[:], in0=su[:], scalar1=cs_ap, scalar2=None, op0=ALU.mult), [t_su, t_csk], [t_hbf])

        def stage_CD(k, wts):
            for fc in range(4):
                Q.op("pe", lambda h, fc=fc: h.transpose(tbv[:, fc * 128:(fc + 1) * 128], hbf[:, fc * 128:(fc + 1) * 128], identb), [t_hbf, s_c], [sbk[4]])
            Q.op("act", lambda h: h.activation(out=hT[k][:], in_=tbv[:, 0:512].rearrange("p (a c) -> p a c", a=4), func=AF.Copy), [sbk[4]], [t_hT[k]])
            for og in range(4):
                wv, t_w = wts[2][og]
                bank = 5 + rot[0] % 3
                rot[0] += 1
                for fc in range(4):
                    Q.op("pe", lambda h, fc=fc, wv=wv, bank=bank: h.matmul(banks[bank][:], lhsT=hT[k][:, fc, :], rhs=wv[:, fc, :], start=(fc == 0), stop=(fc == 3)),
                         [t_hT[k], t_w], [sbk[bank]])
                Q.op("act", lambda h, og=og, bank=bank: h.activation(out=yg[k][:, og * 512:(og + 1) * 512], in_=banks[bank][:], func=AF.Copy), [sbk[bank]], [t_yg[k]])

        def stage_E(k, scat):
            for ot in range(NKC):
                for (rhs_ap, t_rhs, tt, col0, n) in scat:
                    bank = 5 + rot[0] % 3
                    rot[0] += 1
                    Q.op("pe", lambda h, ot=ot, rhs_ap=rhs_ap, bank=bank, n=n: h.matmul(banks[bank][:, 0:n], lhsT=yg[k][:, ot * 128:(ot + 1) * 128], rhs=rhs_ap, start=True, stop=True),
                         [t_yg[k], t_rhs], [sbk[bank]])
                    Q.op("dve", lambda h, ot=ot, bank=bank, n=n, col0=col0: h.tensor_tensor(out=yacc[:, ot, col0:col0 + n], in0=yacc[:, ot, col0:col0 + n], in1=banks[bank][:, 0:n], op=ALU.add),
                         [s_y[ot][tt], sbk[bank]], [s_y[ot][tt]])

        def load_gu(e):
            if e == 0:
                tl = []
                for i_ in range(8):
                    if i_ < N_PRE:
                        a_, b_, c_, tk_ = ring["tiles"][ring["i"] % len(ring["tiles"])]
                        ring["i"] += 1
                        tl.append((b_, tk_))
                    else:
                        tl.append(load_w_q(Q, ring, (wg_d if i_ < 4 else wu_d)[i_ % 4], view=1))
                return [tl[0:4], tl[4:8], None]
            wg_t = [load_w_q(Q, ring, wg_d[e * 4 + q], view=1) for q in range(4)]
            wu_t = [load_w_q(Q, ring, wu_d[e * 4 + q], view=1) for q in range(4)]
            return [wg_t, wu_t, None]

        def load_d(e, w):
            w[2] = [load_w_q(Q, ring, wd_d[e * 4 + q], view=1) for q in range(4)]

        if sparse:
            Sel3 = Sel + [A.at(SC + 31872, [128, 8, 128], BF16)]
            t_Sel3 = t_Sel + [Tk("Sel2")]
            SelT3 = SelT + [A.at(SC + 18432, [128, OWN], BF16)]
            t_SelT3 = t_SelT + [Tk("SelT2")]

            def stage_S(e):
                j3 = e % 3
                for tb in range(8):
                    Q.op("dve", lambda h, tb=tb, e=e, j3=j3: h.tensor_scalar(out=Sel3[j3][:, tb, :], in0=iota1, scalar1=rankm[:, tb, e:e + 1], scalar2=None, op0=ALU.is_equal),
                         [s_c], [t_Sel3[j3]])

            def stage_A(e):
                k = e % 2
                j3 = e % 3
                Sl, t_Sl = Sel3[j3], t_Sel3[j3]
                for tb in range(8):
                    Q.op("pe", lambda h, tb=tb, Sl=Sl: h.transpose(tbv[:, tb * 128:(tb + 1) * 128], Sl[:, tb, :], identb), [t_Sl, s_c], [sbk[4]])
                Q.op("act", lambda h, j3=j3: h.activation(out=SelT3[j3][:], in_=tbv[:, 0:1024], func=AF.Copy), [sbk[4]], [t_SelT3[j3]])
                for tb in range(8):
                    Q.op("pe", lambda h, tb=tb, e=e, Sl=Sl: h.matmul(banks[4][:, 0:2], lhsT=Sl[:, tb, :], rhs=combhl[:, tb, e, :], start=(tb == 0), stop=(tb == 7)),
                         [t_Sl, s_c], [sbk[4]])
                Q.op("dve", lambda h, k=k: h.reduce_sum(out=cs[k][:, 0:1], in_=banks[4][:, 0:2], axis=AX.X), [sbk[4]], [t_cs[k]])
                for q in range(4):
                    bank = q % 2
                    for kk in range(4):
                        kc = 4 * q + kk
                        for tb in range(8):
                            Q.op("pe", lambda h, tb=tb, kc=kc, kk=kk, bank=bank, Sl=Sl: h.matmul(banks[bank][:, kk * 128:(kk + 1) * 128], lhsT=x1tm[:, tb, kc * 128:(kc + 1) * 128],
                                                                                          rhs=Sl[:, tb, :], start=(tb == 0), stop=(tb == 7)),
                                 [s_x1tm, t_Sl], [sbk[bank]])
                    Q.op("act", lambda h, q=q, bank=bank, k=k: h.activation(out=xg[k][:, 4 * q:4 * q + 4, :], in_=banks[bank][:].rearrange("p (a c) -> p a c", a=4), func=AF.Copy),
                         [sbk[bank]], [t_xg[k]])

            def sB(e, w):
                k = e % 2
                stage_B(k, (lambda kc, k=k: xg[k][:, kc, :]), t_xg[k], cs[k][:, 0:1], t_cs[k], w)

            def sE2(e0, e1):
                for ot in range(NKC):
                    for tt in range(2):
                        bank = 5 + rot[0] % 3
                        rot[0] += 1
                        for n_, e_ in enumerate((e0, e1)):
                            k_, j_ = e_ % 2, e_ % 3
                            Q.op("pe", lambda h, ot=ot, tt=tt, bank=bank, k_=k_, j_=j_, n_=n_: h.matmul(
                                banks[bank][:], lhsT=yg[k_][:, ot * 128:(ot + 1) * 128], rhs=SelT3[j_][:, tt * 512:(tt + 1) * 512], start=(n_ == 0), stop=(n_ == 1)),
                                [t_yg[k_], t_SelT3[j_]], [sbk[bank]])
                        Q.op("dve", lambda h, ot=ot, tt=tt, bank=bank: h.tensor_tensor(out=yacc[:, ot, tt * 512:(tt + 1) * 512], in0=yacc[:, ot, tt * 512:(tt + 1) * 512],
                                                                                 in1=banks[bank][:], op=ALU.add),
                             [s_y[ot][tt], sbk[bank]], [s_y[ot][tt]])

            W = {}
            W[0] = load_gu(0)
            load_d(0, W[0])
            stage_S(0)
            stage_S(1)
            stage_A(0)
            sB(0, W[0])
            W[1] = load_gu(1)
            for i in range(NE):
                if i + 2 < NE:
                    stage_S(i + 2)
                if i + 1 < NE:
                    stage_A(i + 1)
                stage_CD(i % 2, W[i])
                if i + 1 < NE:
                    load_d(i + 1, W[i + 1])
                    sB(i + 1, W[i + 1])
                    if i + 2 < NE:
                        W[i + 2] = load_gu(i + 2)
                if i % 2 == 1:
                    sE2(i - 1, i)
        else:
            for e in range(NE):
                w = load_gu(e)
                load_d(e, w)
                for tb in range(8):
                    k = tb % 2
                    stage_B(k, (lambda kc, tb=tb: x1b[:, kc, tb * 128:(tb + 1) * 128]), s_x1b, comb[:, tb, e:e + 1], s_c, w)
                    stage_CD(k, w)
                    stage_E(k, [(identb, s_c, tb // 4, tb * 128, 128)])
        fin = Q.op("dve", lambda h: h.memset(sm[:, 24:25], 0.0), [t for r in s_y for t in r], [Tk("fin")], signal=True)
        return fin

    SP = Prog(nc)
    fin_sp = build_sub(SP, True)
    SP.prepare()
    DN = Prog(nc, esem=SP.esem)
    fin_dn = build_sub(DN, False)
    DN.prepare()

    ALL = bk + t_x1b + [t for r in t_y for t in r] + [t[3] for t in ringM["tiles"]] + t_lt + t_lb + t_et \
        + [t_comb, t_rankm, t_combhl, t_cmf, t_cmb, t_sm, t_pTb, t_mask, t_maskb]
    t_gate = Tk("gate")
    t_blk = {e: Tk(f"blk_{e}") for e in ("act", "dve", "pool", "pe")}
    P.op("dve", lambda h: h.memset(sm[:, 23:24], 0.0), ALL, ALL + [t_gate])

    def blockfn(e):
        def fn(h):
            with h.register(f"flg_{e}") as r:
                h.reg_load(r, flagi[0:1, 0:1])
                with h.If_ne(r, 0):
                    DN.run_engine(e, h)
                    h.wait_ge(fin_dn.ticket[0], fin_dn.ticket[1])
                with h.Else():
                    SP.run_engine(e, h)
                    h.wait_ge(fin_sp.ticket[0], fin_sp.ticket[1])
            return h.nop()
        return fn

    for e in ("pool", "pe", "act", "dve"):
        P.op(e, blockfn(e), [t_gate, t_flag], [t_blk[e]])
    P.op("dve", lambda h: h.memset(sm[:, 22:23], 0.0), list(t_blk.values()), ALL)

    if dbg == "z2":
        o = P.op("sync", lambda h: h.dma_start(out=dbg_d, in_=yacc[:]), [t for r in t_y for t in r], [Tk("dbgo")], dma=True)
        print("ops", P.emit(final_wait_ops=[o.idx]), "sub", {e: (len(SP.per_eng[e]), len(DN.per_eng[e])) for e in ENGS})
        return nc

    x2b = x1b
    t_x2b = [Tk("x2b0"), Tk("x2b1")]
    P.op("dve", lambda h: h.memset(sm[:, 27:28], 0.0), t_x1b, t_x2b)
    for tt in range(2):
        for ot in range(NKC):
            ln_stat(yacc, t_y, ot, tt)
    P.op("pool", lambda h: h.dma_start(out=pTb[:], in_=pT_d), writes=[t_pTb], dma=True)
    outs = []
    t_out = [Tk(f"out{i}") for i in range(8)]
    eti = [0]
    for tt in range(2):
        ln_apply(yacc, t_y, tt, C_G2, C_B2, x2b, t_x2b, acc=False)
    for tt in range(2):
        cs = slice(tt * 512, (tt + 1) * 512)
        for ot in range(NKC):
            wpg, t_wpg = load_w_q(P, ringM, wpg_d[ot])
            wpp, t_wpp = load_w_q(P, ringM, wpp_d[ot], view=0, n=256)
            bg = 0 + ot % 2
            bp = 2 + ot % 2
            for kc in range(NKC):
                P.op("pe", lambda h, kc=kc, bg=bg, cs=cs, wpg=wpg: h.matmul(banks[bg][:], lhsT=wpg[:, kc, :], rhs=x2b[:, kc, cs], start=(kc == 0), stop=(kc == NKC - 1)),
                     [t_wpg, t_x2b[tt]], [bk[bg]])
            for kc in range(2):
                P.op("pe", lambda h, kc=kc, bp=bp, cs=cs, wpp=wpp: h.matmul(banks[bp][:], lhsT=wpp[:, kc, :], rhs=pTb[:, kc, cs], start=(kc == 0), stop=(kc == 1)),
                     [t_wpp, t_pTb], [bk[bp]])
            k = eti[0] % 2
            eti[0] += 1
            s_, t_s = et[k], t_et[k]
            u_, t_u = et[2 + k], t_et[2 + k]
            P.op("act", lambda h, bg=bg, s_=s_: h.activation(out=s_[:], in_=banks[bg][:], func=AF.Sigmoid), [bk[bg]], [t_s])
            P.op("dve", lambda h, bp=bp, s_=s_, u_=u_: h.tensor_tensor(out=u_[:], in0=banks[bp][:], in1=s_[:], op=ALU.mult), [bk[bp], t_s], [t_u])
            P.op("dve", lambda h, ot=ot, cs=cs, u_=u_: h.tensor_tensor(out=yacc[:, ot, cs], in0=yacc[:, ot, cs], in1=u_[:], op=ALU.add), [t_y[ot][tt], t_u], [t_y[ot][tt]])
            if ot % 4 == 3:
                g = ot // 4
                o = P.op("sync", lambda h, g=g, cs=cs: h.dma_start(out=out_d[:, g * 4:(g + 1) * 4, cs], in_=yacc[:, g * 4:(g + 1) * 4, cs]),
                         [t_y[o_][tt] for o_ in range(g * 4, g * 4 + 4)], [t_out[tt * 4 + g]], dma=True)
                outs.append(o.idx)
    info = P.emit(final_wait_ops=outs)
    print("ops", info, "sub", {e: (len(SP.per_eng[e]), len(DN.per_eng[e])) for e in ENGS})
    return nc


def _tile_w(w, ncol):
    K, N = w.shape
    return np.ascontiguousarray(w.reshape(K // 128, 128, N // ncol, ncol).transpose(2, 1, 0, 3))


def _consts(j):
    p = np.arange(128)
    cv = np.zeros((128, 80), np.float32)
    cv[:, 0] = THETA ** (-(p % 32) / 32.0)
    cv[:, 1] = THETA ** (-(p % 64) / 64.0)
    sg64 = np.where((p // 32) % 2 == 0, 1.0, -1.0)
    sg128 = np.where((p // 64) % 2 == 0, 1.0, -1.0)
    cv[:, 2] = 2 * math.pi * sg64
    cv[:, 3] = 2 * math.pi * sg128
    cv[:, 4] = float(j)
    cv[:, 74] = 2 * math.pi
    lg = np.log(1.0 - 2.0 ** (-5.0 - np.arange(4, dtype=np.float64)))
    n = np.arange(128, dtype=np.float64)
    scale = 128.0 ** -0.5
    for h in range(4):
        cv[:, 70 + h] = scale * np.exp((127.0 - n) * lg[h])
    cm = np.zeros((128, 14, 128), np.float32)
    pp_ = np.arange(128)
    cm[2 * (pp_ % 32), 12, pp_] = 1.0
    cm[2 * (pp_ % 32), 13, pp_] = np.where((pp_ // 32) % 2 == 0, 1.0, -1.0)
    cm[:, 11, :] = np.arange(1, 129, dtype=np.float32)[None, :]
    cm[:, 0, :] = np.eye(128)
    cm[:, 1, :] = (n[None, :] >= n[:, None])
    cm[:, 2, :] = 1.0
    rel = n[None, :] - n[:, None]
    for h in range(4):
        cm[:, 3 + h, :] = np.where(rel >= 0, np.exp(rel * lg[h]), 0.0) * scale
        cm[:, 7 + h, :] = np.exp((n + 1.0) * lg[h])[None, :]
    return cv, cm


_NC_CACHE = {}


def _prep_shared(inp):
    f = lambda k: np.asarray(inp[k], np.float32)
    sh = {}
    sh["w_in_r"] = _tile_w(f("w_in")[0], 128).reshape(48, 128, 2048)
    sh["w_out_r"] = _tile_w(f("w_out")[0], 128).reshape(16, 128, 2048)
    wg = f("w_exp_gate")[0]
    wu = f("w_exp_up")[0]
    wd = f("w_exp_down")[0]
    sh["wg_r"] = np.ascontiguousarray(wg.reshape(NE, 4, 4, 128, 512).transpose(0, 1, 3, 2, 4)).reshape(NE * 4, 128, 2048)
    sh["wu_r"] = np.ascontiguousarray(wu.reshape(NE, 4, 4, 128, 512).transpose(0, 1, 3, 2, 4)).reshape(NE * 4, 128, 2048)
    sh["wd_r"] = np.ascontiguousarray(wd.reshape(NE, 4, 128, 4, 512).transpose(0, 3, 2, 1, 4)).reshape(NE * 4, 128, 2048)
    wr = np.concatenate([f("w_router_group")[0], f("w_router_expert")[0]], axis=1)
    sh["wr_r"] = np.ascontiguousarray(wr.reshape(16, 128, 36).transpose(1, 0, 2))
    sh["wpg_r"] = _tile_w(f("w_ple_gate")[0], 128).reshape(16, 128, 2048)
    sh["wpp_r"] = _tile_w(f("w_ple_proj")[0], 128).reshape(16, 128, 256)
    rbv = np.concatenate([f("b_router_group")[0], f("b_router_expert")[0]])
    sh["rb"] = np.ascontiguousarray(np.broadcast_to(rbv[None, :], (128, 36)))
    lv = np.stack([f("da_lambda_q1")[0], f("da_lambda_k1")[0], f("da_lambda_q2")[0], f("da_lambda_k2")[0]])
    sh["lamv"] = np.ascontiguousarray(np.broadcast_to(lv[None], (128, 4, 64)))
    return sh


def _core_maps(inp, dbg=None):
    x = np.asarray(inp["x"], np.float32)
    pp = np.asarray(inp["p"], np.float32)[0]
    pos = np.asarray(inp["positions"]).astype(np.int32)
    sh = _prep_shared(inp)
    vecs = {k: np.asarray(inp[k], np.float32)[0] for k in ("ln1_g", "ln1_b", "ln2_g", "ln2_b", "da_subln_w")}
    maps = []
    for c in range(8):
        b, j = c // 2, c % 2
        own_blocks = [2 * m + j for m in range(8)]
        oth_blocks = [2 * m + j - 1 for m in range(8)]
        xb = x[b].reshape(16, 128, D)
        pb = pos[b].reshape(16, 128)
        xs = np.zeros((16, 128, D), np.float32)
        ps = np.zeros((16, 128), np.int32)
        for m in range(8):
            xs[m] = xb[own_blocks[m]]
            ps[m] = pb[own_blocks[m]]
            if oth_blocks[m] >= 0:
                xs[8 + m] = xb[oth_blocks[m]]
                ps[8 + m] = pb[oth_blocks[m]]
        xs = xs.reshape(S, D)
        xT = np.ascontiguousarray(xs.T.reshape(16, 128, S).transpose(1, 0, 2))
        pown = pp[b].reshape(16, 128, 256)[own_blocks].reshape(OWN, 256)
        pT = np.ascontiguousarray(pown.T.reshape(2, 128, OWN).transpose(1, 0, 2))
        cv, cm = _consts(j)
        cv[:, 5] = vecs["da_subln_w"]
        cv[:, 6:22] = vecs["ln1_g"].reshape(16, 128).T
        cv[:, 22:38] = vecs["ln1_b"].reshape(16, 128).T
        cv[:, 38:54] = vecs["ln2_g"].reshape(16, 128).T
        cv[:, 54:70] = vecs["ln2_b"].reshape(16, 128).T
        m_ = dict(sh)
        m_["xT"] = xT
        m_["posb"] = np.ascontiguousarray(np.broadcast_to(ps.reshape(1, S), (128, S)))
        m_["pT"] = pT
        mb = np.zeros((128, 640), np.float32)
        kk = np.arange(128)
        mb[:, 0:128] = np.where(kk[None, :] < kk[:, None], -30000.0, 0.0)
        mb[:, 128:640] = -30000.0 * (1 - j)
        m_["mbias"] = mb
        m_["cvec"] = cv
        m_["cmat"] = cm
        maps.append(m_)
    return maps


def _assemble(res_list, key="outT"):
    out = np.zeros((NB, S, D), np.float32)
    for c in range(8):
        b, j = c // 2, c % 2
        oT = np.asarray(res_list[c][key])
        o = oT.transpose(2, 1, 0).reshape(OWN, D)
        for m in range(8):
            g = 2 * m + j
            out[b, g * 128:(g + 1) * 128] = o[m * 128:(m + 1) * 128]
    return out


def kernel(**inputs):
    if "nc" not in _NC_CACHE:
        _NC_CACHE["nc"] = build()
    nc = _NC_CACHE["nc"]
    maps = _core_maps(inputs)
    res = run_bass_kernel_spmd(nc, maps, core_ids=list(range(8)))
    return _assemble(res.results)
```
